# Optimizing a Trainium2 kernel written in Bass

```python
import math
import jax, jax.numpy as jnp
from jax import lax
import numpy as np

D_MODEL = 2048
BATCH = 4
SEQ = 2048
DEPTH = 4

GRID_W = 64
CTX_LEN = 256
N_MIXERS = 3
BLOCK_Q = 128
CHUNK = 128
EPS = 1e-6
ROPE_THETA = 10000.0

A_WIDTH = D_MODEL
A_GROUPS = D_MODEL // 128
A_GROUP_DIM = A_WIDTH // A_GROUPS

B_HEAD_DIM = 128
B_HEADS = D_MODEL // B_HEAD_DIM
B_KV_HEADS = B_HEADS // 2
B_GQA_GROUP = B_HEADS // B_KV_HEADS
B_Q_WIDTH = B_HEADS * B_HEAD_DIM
B_KV_WIDTH = B_KV_HEADS * B_HEAD_DIM

C_HEAD_DIM = 64
C_HEADS = D_MODEL // (2 * C_HEAD_DIM)
C_V_DIM = 2 * C_HEAD_DIM
C_WIDTH = C_HEADS * 2 * C_HEAD_DIM

N_GROUPS = 4
EXPERTS_PER_GROUP = 8
TOP_K = 2
D_EXPERT = 3 * D_MODEL // 8
ROUTER_BIAS_SCALE = 0.01

N_GMLP_LAYERS = (DEPTH + N_MIXERS - 1) // N_MIXERS
N_GQA_LAYERS = (DEPTH - 1 + N_MIXERS - 1) // N_MIXERS
N_DIFF_LAYERS = (DEPTH - 2 + N_MIXERS - 1) // N_MIXERS

kernel_name = "hybrid_gmlp_gqa_diffattn_hmoe_dit"


def rms_norm(x, gain):
    xf = x.astype(jnp.float32)
    y = xf * lax.rsqrt(jnp.mean(xf * xf, axis=-1, keepdims=True) + EPS)
    return (y * gain.astype(jnp.float32)).astype(x.dtype)


def modulate(xn, shift, scale):
    return xn * (1 + scale) + shift


def axial_rope_tables(n_tokens, head_dim):
    rows = n_tokens // GRID_W
    row_id = jnp.repeat(jnp.arange(rows, dtype=jnp.float32), GRID_W)
    col_id = jnp.tile(jnp.arange(GRID_W, dtype=jnp.float32), rows)
    half = head_dim // 2
    inv_freq = ROPE_THETA ** (-jnp.arange(0, half, 2, dtype=jnp.float32) / half)
    ang_r = row_id[:, None] * inv_freq[None, :]
    ang_c = col_id[:, None] * inv_freq[None, :]
    ang = jnp.concatenate([ang_r, ang_r, ang_c, ang_c], axis=-1)
    return jnp.cos(ang), jnp.sin(ang)


def apply_axial_rope(x, cos, sin):
    hd = x.shape[-1]
    bshape = (cos.shape[0],) + (1,) * (x.ndim - 3) + (hd,)
    cos = cos.reshape(bshape).astype(x.dtype)
    sin = sin.reshape(bshape).astype(x.dtype)
    r1, r2, c1, c2 = jnp.split(x, 4, axis=-1)
    rot = jnp.concatenate([-r2, r1, -c2, c1], axis=-1)
    return x * cos + rot * sin


def sweep_query_blocks(fn, q):
    bsz, n = q.shape[:2]
    qb = q.reshape((bsz, n // BLOCK_Q, BLOCK_Q) + q.shape[2:]).swapaxes(0, 1)
    ob = lax.map(fn, qb)
    ob = ob.swapaxes(0, 1)
    return ob.reshape((bsz, n) + ob.shape[3:])


def chunk_spatial_gating(h, w_in, v_gain, w_s, b_s):
    bsz, n, _ = h.shape
    z = jax.nn.gelu(h @ w_in)
    u, v = jnp.split(z, 2, axis=-1)
    v = rms_norm(v, v_gain).reshape(bsz, n // CHUNK, CHUNK, A_GROUPS, A_GROUP_DIM)
    s = jnp.einsum('gpq,bcqge->bcpge', w_s, v) + b_s.T[:, :, None]
    return u * s.reshape(bsz, n, A_WIDTH)


def mixer_gmlp(hc, hx, w_in, v_gain, w_s, b_s, w_out, need_ctx):
    yx = chunk_spatial_gating(hx, w_in, v_gain, w_s, b_s) @ w_out
    yc = chunk_spatial_gating(hc, w_in, v_gain, w_s, b_s) @ w_out if need_ctx else None
    return yc, yx


def gqa_softmax(q, k, v):
    s = jnp.einsum('bqhgd,bkhd->bhgqk', q, k).astype(jnp.float32) / math.sqrt(q.shape[-1])
    p = jax.nn.softmax(s, axis=-1).astype(v.dtype)
    return jnp.einsum('bhgqk,bkhd->bqhgd', p, v)


def mixer_gqa(hc, hx, w_qkv, q_gain, k_gain, w_o, cos, sin, need_ctx):
    def project(h):
        bsz, n, _ = h.shape
        q, k, v = jnp.split(h @ w_qkv, [B_Q_WIDTH, B_Q_WIDTH + B_KV_WIDTH], axis=-1)
        q = rms_norm(q.reshape(bsz, n, B_KV_HEADS, B_GQA_GROUP, B_HEAD_DIM), q_gain)
        k = rms_norm(k.reshape(bsz, n, B_KV_HEADS, B_HEAD_DIM), k_gain)
        v = v.reshape(bsz, n, B_KV_HEADS, B_HEAD_DIM)
        return q, k, v

    qc, kc, vc = project(hc)
    qx, kx, vx = project(hx)
    qx = apply_axial_rope(qx, cos, sin)
    kx = apply_axial_rope(kx, cos, sin)
    k_all = jnp.concatenate([kc, kx], axis=1)
    v_all = jnp.concatenate([vc, vx], axis=1)
    ox = sweep_query_blocks(lambda qb: gqa_softmax(qb, k_all, v_all), qx)
    yx = ox.reshape(hx.shape[0], hx.shape[1], B_Q_WIDTH) @ w_o
    yc = None
    if need_ctx:
        oc = gqa_softmax(qc, kc, vc)
        yc = oc.reshape(hc.shape[0], hc.shape[1], B_Q_WIDTH) @ w_o
    return yc, yx


def mixer_diff(hc, hx, w_qkv, q_gain, k_gain, lam_q1, lam_k1, lam_q2, lam_k2, sub_gain, w_o,
               cos, sin, lam_init, need_ctx):
    def project(h):
        bsz, n, _ = h.shape
        q, k, v = jnp.split(h @ w_qkv, 3, axis=-1)
        q = rms_norm(q.reshape(bsz, n, C_HEADS, 2, C_HEAD_DIM), q_gain)
        k = rms_norm(k.reshape(bsz, n, C_HEADS, 2, C_HEAD_DIM), k_gain)
        return q, k, v.reshape(bsz, n, C_HEADS, C_V_DIM)

    f32 = jnp.float32
    lam = (jnp.exp(jnp.sum(lam_q1.astype(f32) * lam_k1.astype(f32)))
           - jnp.exp(jnp.sum(lam_q2.astype(f32) * lam_k2.astype(f32))) + lam_init)

    def attend(q, k, v):
        s = jnp.einsum('bqhmd,bkhmd->bhmqk', q, k).astype(f32) / math.sqrt(C_HEAD_DIM)
        p = jax.nn.softmax(s, axis=-1)
        a = (p[:, :, 0] - lam * p[:, :, 1]).astype(v.dtype)
        return jnp.einsum('bhqk,bkhe->bqhe', a, v)

    def finish(o):
        o = rms_norm(o, sub_gain) * (1.0 - lam_init)
        return o.reshape(o.shape[0], o.shape[1], C_HEADS * C_V_DIM) @ w_o

    qc, kc, vc = project(hc)
    qx, kx, vx = project(hx)
    qx = apply_axial_rope(qx, cos, sin)
    kx = apply_axial_rope(kx, cos, sin)
    k_all = jnp.concatenate([kc, kx], axis=1)
    v_all = jnp.concatenate([vc, vx], axis=1)
    yx = finish(sweep_query_blocks(lambda qb: attend(qb, k_all, v_all), qx))
    yc = finish(attend(qc, kc, vc)) if need_ctx else None
    return yc, yx


def hierarchical_moe(t, w_grp, b_grp, w_exp, b_exp, w_gate, w_up, w_down):
    f32 = jnp.float32
    n_tok = t.shape[0]
    grp_logits = (t @ w_grp).astype(f32) + b_grp.astype(f32)
    grp_prob = jax.nn.softmax(grp_logits, axis=-1)
    grp_idx = jnp.argmax(grp_logits, axis=-1)
    grp_onehot = jax.nn.one_hot(grp_idx, N_GROUPS, dtype=f32)
    grp_w = jnp.sum(grp_prob * grp_onehot, axis=-1)
    exp_logits = ((t @ w_exp).astype(f32) + b_exp.astype(f32)).reshape(n_tok, N_GROUPS, EXPERTS_PER_GROUP)
    sel_logits = jnp.take_along_axis(exp_logits, grp_idx[:, None, None], axis=1)[:, 0]
    top_v, top_i = lax.top_k(sel_logits, TOP_K)
    top_w = jax.nn.softmax(top_v, axis=-1)
    in_grp = jnp.sum(jax.nn.one_hot(top_i, EXPERTS_PER_GROUP, dtype=f32) * top_w[..., None], axis=1)
    combine = (grp_w[:, None, None] * grp_onehot[:, :, None] * in_grp[:, None, :]).astype(t.dtype)
    out = jnp.zeros_like(t)
    for g in range(N_GROUPS):
        a = jnp.einsum('td,edf->tef', t, w_gate[g])
        b = jnp.einsum('td,edf->tef', t, w_up[g])
        out = out + jnp.einsum('tef,efd->td', jax.nn.silu(a) * b * combine[:, g, :, None], w_down[g])
    return out


def setup_inputs(seed: int = 0) -> dict:
    key = jax.random.key(seed)
    ks = iter(jax.random.split(key, 64))
    d = D_MODEL

    def nrm(shape, scale):
        return jax.random.normal(next(ks), shape, jnp.float32) * scale

    def gain(shape):
        return 1.0 + nrm(shape, 0.05)

    return {
        "x": nrm((BATCH, SEQ, d), 1.0),
        "c": nrm((BATCH, d), 1.0),
        "ctx": nrm((BATCH, CTX_LEN, d), 1.0),
        "c_ctx": nrm((d,), 1.0),
        "ada_w": nrm((DEPTH, d, 6 * d), 0.5 * d ** -0.5),
        "ada_b": nrm((DEPTH, 6 * d), 0.02),
        "norm1_g": gain((DEPTH, d)),
        "norm2_g": gain((DEPTH, d)),
        "gmlp_w_in": nrm((N_GMLP_LAYERS, d, 2 * A_WIDTH), d ** -0.5),
        "gmlp_v_gain": gain((N_GMLP_LAYERS, A_WIDTH)),
        "gmlp_w_s": nrm((N_GMLP_LAYERS, A_GROUPS, CHUNK, CHUNK), CHUNK ** -0.5),
        "gmlp_b_s": gain((N_GMLP_LAYERS, A_GROUPS, CHUNK)),
        "gmlp_w_out": nrm((N_GMLP_LAYERS, A_WIDTH, d), A_WIDTH ** -0.5),
        "gqa_w_qkv": nrm((N_GQA_LAYERS, d, B_Q_WIDTH + 2 * B_KV_WIDTH), d ** -0.5),
        "gqa_q_gain": gain((N_GQA_LAYERS, B_HEAD_DIM)),
        "gqa_k_gain": gain((N_GQA_LAYERS, B_HEAD_DIM)),
        "gqa_w_o": nrm((N_GQA_LAYERS, B_Q_WIDTH, d), B_Q_WIDTH ** -0.5),
        "diff_w_qkv": nrm((N_DIFF_LAYERS, d, 3 * C_WIDTH), d ** -0.5),
        "diff_q_gain": gain((N_DIFF_LAYERS, 2, C_HEAD_DIM)),
        "diff_k_gain": gain((N_DIFF_LAYERS, 2, C_HEAD_DIM)),
        "diff_lam_q1": nrm((N_DIFF_LAYERS, C_HEAD_DIM), 0.1),
        "diff_lam_k1": nrm((N_DIFF_LAYERS, C_HEAD_DIM), 0.1),
        "diff_lam_q2": nrm((N_DIFF_LAYERS, C_HEAD_DIM), 0.1),
        "diff_lam_k2": nrm((N_DIFF_LAYERS, C_HEAD_DIM), 0.1),
        "diff_sub_gain": gain((N_DIFF_LAYERS, C_V_DIM)),
        "diff_w_o": nrm((N_DIFF_LAYERS, C_HEADS * C_V_DIM, d), (C_HEADS * C_V_DIM) ** -0.5),
        "moe_w_grp": nrm((DEPTH, d, N_GROUPS), d ** -0.5),
        "moe_b_grp": nrm((DEPTH, N_GROUPS), ROUTER_BIAS_SCALE),
        "moe_w_exp": nrm((DEPTH, d, N_GROUPS * EXPERTS_PER_GROUP), d ** -0.5),
        "moe_b_exp": nrm((DEPTH, N_GROUPS * EXPERTS_PER_GROUP), ROUTER_BIAS_SCALE),
        "moe_w_gate": nrm((DEPTH, N_GROUPS, EXPERTS_PER_GROUP, d, D_EXPERT), d ** -0.5),
        "moe_w_up": nrm((DEPTH, N_GROUPS, EXPERTS_PER_GROUP, d, D_EXPERT), d ** -0.5),
        "moe_w_down": nrm((DEPTH, N_GROUPS, EXPERTS_PER_GROUP, D_EXPERT, d), D_EXPERT ** -0.5),
    }


def reference(x, c, ctx, c_ctx, ada_w, ada_b, norm1_g, norm2_g,
              gmlp_w_in, gmlp_v_gain, gmlp_w_s, gmlp_b_s, gmlp_w_out,
              gqa_w_qkv, gqa_q_gain, gqa_k_gain, gqa_w_o,
              diff_w_qkv, diff_q_gain, diff_k_gain, diff_lam_q1, diff_lam_k1, diff_lam_q2, diff_lam_k2,
              diff_sub_gain, diff_w_o,
              moe_w_grp, moe_b_grp, moe_w_exp, moe_b_exp, moe_w_gate, moe_w_up, moe_w_down):
    bsz, n_lat, d = x.shape
    n_ctx = ctx.shape[1]
    cos_b, sin_b = axial_rope_tables(n_lat, B_HEAD_DIM)
    cos_c, sin_c = axial_rope_tables(n_lat, C_HEAD_DIM)
    hx, hc = x, ctx
    for i in range(DEPTH):
        kind = i % N_MIXERS
        j = i // N_MIXERS
        need_ctx = any((k % N_MIXERS) != 0 for k in range(i + 1, DEPTH))
        sh1, sc1, g1, sh2, sc2, g2 = jnp.split(jax.nn.silu(c) @ ada_w[i] + ada_b[i], 6, axis=-1)
        csh1, csc1, cg1, csh2, csc2, cg2 = jnp.split(jax.nn.silu(c_ctx) @ ada_w[i] + ada_b[i], 6, axis=-1)

        ax = modulate(rms_norm(hx, norm1_g[i]), sh1[:, None], sc1[:, None])
        ac = modulate(rms_norm(hc, norm1_g[i]), csh1, csc1)
        if kind == 0:
            yc, yx = mixer_gmlp(ac, ax, gmlp_w_in[j], gmlp_v_gain[j], gmlp_w_s[j], gmlp_b_s[j],
                                gmlp_w_out[j], need_ctx)
        elif kind == 1:
            yc, yx = mixer_gqa(ac, ax, gqa_w_qkv[j], gqa_q_gain[j], gqa_k_gain[j], gqa_w_o[j],
                               cos_b, sin_b, need_ctx)
        else:
            lam_init = 0.8 - 0.6 * math.exp(-0.3 * i)
            yc, yx = mixer_diff(ac, ax, diff_w_qkv[j], diff_q_gain[j], diff_k_gain[j],
                                diff_lam_q1[j], diff_lam_k1[j], diff_lam_q2[j], diff_lam_k2[j],
                                diff_sub_gain[j], diff_w_o[j], cos_c, sin_c, lam_init, need_ctx)
        hx = hx + g1[:, None] * yx

        mx = modulate(rms_norm(hx, norm2_g[i]), sh2[:, None], sc2[:, None])
        moe_args = (moe_w_grp[i], moe_b_grp[i], moe_w_exp[i], moe_b_exp[i],
                    moe_w_gate[i], moe_w_up[i], moe_w_down[i])
        if need_ctx:
            hc = hc + cg1 * yc
            mc = modulate(rms_norm(hc, norm2_g[i]), csh2, csc2)
            tokens = jnp.concatenate([mc, mx], axis=1).reshape(-1, d)
            y = hierarchical_moe(tokens, *moe_args).reshape(bsz, n_ctx + n_lat, d)
            hc = hc + cg2 * y[:, :n_ctx]
            yx2 = y[:, n_ctx:]
        else:
            yx2 = hierarchical_moe(mx.reshape(-1, d), *moe_args).reshape(bsz, n_lat, d)
        hx = hx + g2[:, None] * yx2
    return hx
```

```python
import contextlib
import numpy as np
import ml_dtypes
import concourse.bass as bass
import concourse.mybir as mybir
from concourse.bass_utils import run_bass_kernel_spmd

F32 = mybir.dt.float32
BF16 = mybir.dt.bfloat16
AF = mybir.ActivationFunctionType
ALU = mybir.AluOpType
AX = mybir.AxisListType
NCORES = 8
D = 2048
KC = 16
EPS = 1e-6


class Tok:
    __slots__ = ("w", "r")

    def __init__(self):
        self.w = []
        self.r = {}


class Prog:
    ENG = ("pe", "act", "dve", "pool", "sp")

    def __init__(self, n_dma_sems=10):
        self.nc = bass.Bass("TRN2", target_bir_lowering=False)
        self.es = contextlib.ExitStack()
        self.sems = {}
        self.cnt = {}
        self.known = {e: {} for e in self.ENG}
        self.q = {e: [] for e in self.ENG}
        for e in self.ENG:
            self._mk("e_" + e)
        self.dsem = ["d%d" % i for i in range(n_dma_sems)]
        for s in self.dsem:
            self._mk(s)
        self.dnext = 0
        self.out_toks = []
        self.n_t = 0

    def _mk(self, name):
        self.sems[name] = self.es.enter_context(self.nc.semaphore(name))
        self.cnt[name] = 0

    def dram_in(self, name, shape, dt=F32):
        return self.nc.dram_tensor(name, list(shape), dt, kind="ExternalInput").ap()

    def dram_out(self, name, shape, dt=F32):
        return self.nc.dram_tensor(name, list(shape), dt, kind="ExternalOutput").ap()

    def dram(self, name, shape, dt=F32):
        return self.nc.dram_tensor(name, list(shape), dt).ap()

    def sb(self, shape, dt=F32, name=None):
        self.n_t += 1
        return self.es.enter_context(self.nc.sbuf_tensor(name or "t%d" % self.n_t, list(shape), dt))

    def ps(self, shape=(128, 512), dt=F32, name=None):
        self.n_t += 1
        return self.es.enter_context(self.nc.psum_tensor(name or "p%d" % self.n_t, list(shape), dt))

    def _need(self, eng, waits, tok):
        if tok is None:
            return
        s, v = tok
        if eng == "pe" and s == "e_pe":
            return
        if self.known[eng].get(s, 0) < v:
            waits[s] = max(waits.get(s, 0), v)

    def _deps(self, eng, reads, writes):
        waits = {}
        for o in reads:
            for t in o.w:
                self._need(eng, waits, t)
        for o in writes:
            for t in o.w:
                self._need(eng, waits, t)
            for s, v in o.r.items():
                self._need(eng, waits, (s, v))
        for s, v in waits.items():
            self.known[eng][s] = v
        return list(waits.items())

    def op(self, eng, fn, reads=(), writes=(), sig=True):
        waits = self._deps(eng, reads, writes)
        s = "e_" + eng
        if sig:
            self.cnt[s] += 1
            tok = (s, self.cnt[s])
        else:
            tok = (s, self.cnt[s] + 1)
        for o in reads:
            o.r[tok[0]] = max(o.r.get(tok[0], 0), tok[1])
        for o in writes:
            o.w = [tok]
            o.r = {}
        self.q[eng].append((waits, fn, (s, 1) if sig else None))

    def dma(self, queue, out, in_, reads=(), writes=(), join=False, **kw):
        s = self.dsem[self.dnext]
        self.dnext = (self.dnext + 1) % len(self.dsem)
        waits = self._deps(queue, reads, writes)
        prev = self.cnt[s]
        if prev and self.known[queue].get(s, 0) < prev:
            waits.append((s, prev))
            self.known[queue][s] = prev
        self.cnt[s] += 16
        tok = (s, self.cnt[s])
        for o in reads:
            o.r[s] = tok[1]
        for o in writes:
            if join:
                o.w = [t for t in o.w if t[0] != s] + [tok]
            else:
                o.w = [tok]
                o.r = {}
        self.q[queue].append((waits, lambda e: e.dma_start(out=out, in_=in_, **kw), (s, 16)))

    def out_dma(self, queue, out, in_, reads=()):
        t = Tok()
        self.dma(queue, out, in_, reads=reads, writes=(t,))
        self.out_toks.append(t)

    def build(self):
        waits = self._deps("sp", self.out_toks, ())
        self.q["sp"].append((waits, None, None))
        nc = self.nc

        def run(e, name):
            for waits, fn, inc in self.q[name]:
                for s, v in waits:
                    e.wait_ge(self.sems[s], v)
                if fn is None:
                    continue
                ins = fn(e)
                if inc is not None:
                    ins.then_inc(self.sems[inc[0]], inc[1])

        with nc.Block() as block:
            @block.tensor
            def _(e):
                run(e, "pe")

            @block.scalar
            def _(e):
                run(e, "act")

            @block.vector
            def _(e):
                run(e, "dve")

            @block.gpsimd
            def _(e):
                run(e, "pool")

            @block.sync
            def _(e):
                run(e, "sp")
        self.es.close()
        return nc


def launch(prog, in_maps):
    nc = prog.build()
    res = run_bass_kernel_spmd(nc, in_maps, core_ids=list(range(len(in_maps))))
    return res.results


ADA_COLS = 6 * D // NCORES


def build_ada(depth):
    p = Prog()
    cT = p.dram_in("cT", [128, KC, 5])
    w = p.dram_in("w", [depth, D, ADA_COLS])
    b = p.dram_in("b", [depth, 5, ADA_COLS])
    out = p.dram_out("mod", [depth, 5, ADA_COLS])
    c_sb, c_t = p.sb([128, KC, 5]), Tok()
    s_sb, s_t = p.sb([128, KC, 5]), Tok()
    p.dma("sp", c_sb[:], cT, writes=(c_t,))
    p.op("act", lambda e: e.activation(out=s_sb[:], in_=c_sb[:], func=AF.Silu), reads=(c_t,), writes=(s_t,))
    w_sb = p.sb([128, KC, ADA_COLS])
    w_ts = [Tok() for _ in range(4)]
    bb = [(p.sb([5, ADA_COLS]), Tok()) for _ in range(2)]
    ob = [(p.sb([5, ADA_COLS]), Tok()) for _ in range(2)]
    pss = [(p.ps(), Tok()) for _ in range(3)]
    for l in range(depth):
        b_sb, b_t = bb[l % 2]
        o_sb, o_t = ob[l % 2]
        wv = w[l].rearrange("(k p) n -> p k n", p=128)
        for kk in range(0, KC, 4):
            p.dma("sp", w_sb[:, kk:kk + 4, :], wv[:, kk:kk + 4, :], writes=(w_ts[kk // 4],))
        p.dma("sp", b_sb[:], b[l], writes=(b_t,))
        for n in range(3):
            ps_, ps_t = pss[n]
            for k in range(KC):
                p.op("pe", lambda e, k=k, n=n, ps_=ps_: e.matmul(
                    ps_[0:5, :], lhsT=s_sb[:, k, :], rhs=w_sb[:, k, n * 512:(n + 1) * 512],
                    start=(k == 0), stop=(k == KC - 1)),
                    reads=(s_t, w_ts[k // 4]), writes=(ps_t,), sig=(k == KC - 1))
            p.op("dve", lambda e, n=n, ps_=ps_, o_sb=o_sb, b_sb=b_sb: e.tensor_tensor(
                out=o_sb[:, n * 512:(n + 1) * 512], in0=ps_[0:5, :], in1=b_sb[:, n * 512:(n + 1) * 512], op=ALU.add),
                reads=(ps_t, b_t), writes=(o_t,))
        p.out_dma("sp", out[l], o_sb[:], reads=(o_t,))
    return p


GELU_C = 1.5957691216057308


class TP:
    def __init__(self, p, T, n_part):
        self.p = p
        self.T = T
        self.n_lat = 1024
        self.has_ctx = T > 1024
        self.n_part = n_part
        if self.has_ctx:
            self.blocks = [(0, 512, [(0, 512, 0)]), (512, 640, [(0, 512, 0), (512, 128, 1)])]
        else:
            self.blocks = [(0, 512, [(0, 512, 0)]), (512, 512, [(0, 512, 0)])]
        self.TB = 640 if self.has_ctx else 512
        self.h_in = p.dram_in("h_in", [128, KC, T])
        self.modv_d = p.dram_in("modv", [128, KC, 12])
        self.ng_d = p.dram_in("ng", [128, KC, 2])
        if n_part:
            self.part_d = p.dram_in("part", [n_part, 128, KC, T])
        self.modv, self.modv_t = p.sb([128, KC, 12]), Tok()
        self.ng, self.ng_t = p.sb([128, KC, 2]), Tok()
        self.gs, self.gs_t = p.sb([128, KC, 4]), Tok()
        p.dma("sp", self.modv[:], self.modv_d, writes=(self.modv_t,))
        p.dma("sp", self.ng[:], self.ng_d, writes=(self.ng_t,))
        for kind in range(2):
            for which in range(2):
                j = kind * 2 + which
                sc = self.modv[:, :, kind * 6 + which * 3 + 1]
                p.op("dve", lambda e, j=j, sc=sc, which=which: e.scalar_tensor_tensor(
                    out=self.gs[:, :, j], in0=sc, scalar=1.0, in1=self.ng[:, :, which], op0=ALU.add, op1=ALU.mult),
                    reads=(self.modv_t, self.ng_t), writes=(self.gs_t,))
        self.eps_c, self.eps_t = p.sb([128, 1]), Tok()
        p.op("dve", lambda e: e.memset(self.eps_c[:], EPS), writes=(self.eps_t,))
        self.ones_bf, self.ones_t = p.sb([128, 128], BF16), Tok()
        p.op("dve", lambda e: e.memset(self.ones_bf[:], 1.0), writes=(self.ones_t,))
        self.pss = [(p.ps(), Tok()) for _ in range(8)]
        self.ps_i = 0
        self.tmps = [(p.sb([128, 512]), Tok()) for _ in range(6)]
        self.tmp_i = 0
        self.hch = [(p.sb([128, self.TB]), Tok()) for _ in range(3)]
        self.hch_i = 0
        self.accs = [(p.sb([128, self.TB]), Tok()) for _ in range(2)] if n_part else []
        self.acc_i = 0
        self.rstd, self.rstd_t = p.sb([128, self.TB]), Tok()
        self.sq = [(p.sb([128, self.TB], BF16), Tok()) for _ in range(2)]
        self.sq_i = 0

    def bank(self):
        r = self.pss[self.ps_i]
        self.ps_i = (self.ps_i + 1) % len(self.pss)
        return r

    def tmp(self):
        r = self.tmps[self.tmp_i]
        self.tmp_i = (self.tmp_i + 1) % len(self.tmps)
        return r

    def hchunk(self):
        r = self.hch[self.hch_i]
        self.hch_i = (self.hch_i + 1) % len(self.hch)
        return r

    def mod(self, kind, m):
        return self.modv[:, :, kind * 6 + m]

    def prologue(self, g2_d=None):
        p = self.p
        if not self.n_part:
            return self.h_in, Tok()
        hcur = p.dram("hcur", [128, KC, self.T])
        hcur_t = Tok()
        g2, g2_t = p.sb([128, KC, 2]), Tok()
        p.dma("sp", g2[:], g2_d, writes=(g2_t,))
        for (b0, tb, sls) in self.blocks:
            for k in range(KC):
                acc, acc_t = self.accs[self.acc_i]
                self.acc_i ^= 1
                p.dma("sp", acc[:, :tb], self.part_d[0, :, k, b0:b0 + tb], writes=(acc_t,))
                for j in range(1, self.n_part):
                    t2, t2_t = self.hchunk()
                    p.dma("sp", t2[:, :tb], self.part_d[j, :, k, b0:b0 + tb], writes=(t2_t,))
                    p.op("pool" if j % 2 else "dve", lambda e, acc=acc, t2=t2, tb=tb: e.tensor_tensor(
                        out=acc[:, :tb], in0=acc[:, :tb], in1=t2[:, :tb], op=ALU.add),
                        reads=(t2_t, acc_t), writes=(acc_t,))
                t2, t2_t = self.hchunk()
                p.dma("sp", t2[:, :tb], self.h_in[:, k, b0:b0 + tb], writes=(t2_t,))
                for (s0, n, kind) in sls:
                    p.op("dve", lambda e, acc=acc, t2=t2, s0=s0, n=n, k=k, kind=kind: e.scalar_tensor_tensor(
                        out=acc[:, s0:s0 + n], in0=acc[:, s0:s0 + n], scalar=g2[:, k, kind:kind + 1],
                        in1=t2[:, s0:s0 + n], op0=ALU.mult, op1=ALU.add),
                        reads=(acc_t, t2_t, g2_t), writes=(acc_t,))
                p.dma("sp", hcur[:, k, b0:b0 + tb], acc[:, :tb], reads=(acc_t,), writes=(hcur_t,), join=True)
        return hcur, hcur_t

    def rms(self, h_d, h_t, b0, tb):
        p = self.p
        ps_, ps_t = self.bank()
        ps2_, ps2_t = self.bank()
        for k in range(KC):
            hc, hc_t = self.hchunk()
            p.dma("sp", hc[:, :tb], h_d[:, k, b0:b0 + tb], reads=(h_t,), writes=(hc_t,))
            sq, sq_t = self.sq[self.sq_i]
            self.sq_i ^= 1
            p.op("act", lambda e, sq=sq, hc=hc, tb=tb: e.activation(out=sq[:, :tb], in_=hc[:, :tb], func=AF.Square),
                 reads=(hc_t,), writes=(sq_t,))
            p.op("pe", lambda e, sq=sq, k=k, ps_=ps_: e.matmul(
                ps_[:, 0:512], lhsT=self.ones_bf[:], rhs=sq[:, 0:512], start=(k == 0), stop=(k == KC - 1)),
                reads=(sq_t, self.ones_t), writes=(ps_t,))
            if tb > 512:
                p.op("pe", lambda e, sq=sq, k=k, ps2_=ps2_, tb=tb: e.matmul(
                    ps2_[:, 0:tb - 512], lhsT=self.ones_bf[:], rhs=sq[:, 512:tb], start=(k == 0), stop=(k == KC - 1)),
                    reads=(sq_t, self.ones_t), writes=(ps2_t,))
        for (pp, pt, c0, n) in ((ps_, ps_t, 0, 512), (ps2_, ps2_t, 512, tb - 512)):
            if n <= 0:
                continue
            p.op("act", lambda e, pp=pp, c0=c0, n=n: e.activation(
                out=self.rstd[:, c0:c0 + n], in_=pp[:, 0:n], func=AF.Sqrt, scale=1.0 / D, bias=self.eps_c[:, 0:1]),
                reads=(pt, self.eps_t), writes=(self.rstd_t,))
            p.op("dve", lambda e, c0=c0, n=n: e.reciprocal(out=self.rstd[:, c0:c0 + n], in_=self.rstd[:, c0:c0 + n]),
                 reads=(self.rstd_t,), writes=(self.rstd_t,))

    def norm_mod(self, h_d, h_t, b0, tb, sls, which, out, out_t):
        p = self.p
        self.rms(h_d, h_t, b0, tb)
        for k in range(KC):
            hc, hc_t = self.hchunk()
            p.dma("sp", hc[:, :tb], h_d[:, k, b0:b0 + tb], reads=(h_t,), writes=(hc_t,))
            p.op("dve", lambda e, hc=hc, tb=tb: e.tensor_tensor(
                out=hc[:, :tb], in0=hc[:, :tb], in1=self.rstd[:, :tb], op=ALU.mult),
                reads=(hc_t, self.rstd_t), writes=(hc_t,))
            for (s0, n, kind) in sls:
                p.op("act", lambda e, hc=hc, s0=s0, n=n, k=k, kind=kind: e.activation(
                    out=out[:, k, s0:s0 + n], in_=hc[:, s0:s0 + n], func=AF.Identity,
                    scale=self.gs[:, k, kind * 2 + which:kind * 2 + which + 1],
                    bias=self.modv[:, k, kind * 6 + which * 3:kind * 6 + which * 3 + 1]),
                    reads=(hc_t, self.gs_t, self.modv_t), writes=(out_t,))

    def gelu(self, ps_ap, ps_t, out_ap, out_t, n):
        p = self.p
        t1, t1_t = self.tmp()
        t2, t2_t = self.tmp()
        p.op("act", lambda e: e.activation(out=t1[:, :n], in_=ps_ap, func=AF.Square), reads=(ps_t,), writes=(t1_t,))
        p.op("dve", lambda e: e.tensor_scalar(out=t1[:, :n], in0=t1[:, :n], scalar1=0.044715, scalar2=1.0,
                                              op0=ALU.mult, op1=ALU.add), reads=(t1_t,), writes=(t1_t,))
        p.op("dve", lambda e: e.tensor_tensor(out=t1[:, :n], in0=t1[:, :n], in1=ps_ap, op=ALU.mult),
             reads=(t1_t, ps_t), writes=(t1_t,))
        p.op("act", lambda e: e.activation(out=t2[:, :n], in_=t1[:, :n], func=AF.Sigmoid, scale=GELU_C),
             reads=(t1_t,), writes=(t2_t,))
        p.op("dve", lambda e: e.tensor_tensor(out=out_ap, in0=t2[:, :n], in1=ps_ap, op=ALU.mult),
             reads=(t2_t, ps_t), writes=(out_t,))

    def load_w(self, w_d, c0, ncols, W, W_ts):
        wv = w_d.rearrange("(k p) n -> p k n", p=128)
        for qi in range(ncols // 512):
            self.p.dma("pool", W[:, :, qi * 512:(qi + 1) * 512], wv[:, :, c0 + qi * 512:c0 + (qi + 1) * 512],
                       writes=(W_ts[qi],))

    def proj_fm(self, W, W_ts, nf, x, x_t, sls, cb, kc=KC, f0=0, col0=0):
        p = self.p
        for f in range(nf):
            cf = col0 + f * 128
            w_t = W_ts[cf // 512]
            for (s0, n, kind) in sls:
                ps_, ps_t = self.bank()
                for k in range(kc):
                    p.op("pe", lambda e, cf=cf, k=k, s0=s0, n=n, ps_=ps_: e.matmul(
                        ps_[:, 0:n], lhsT=W[:, k, cf:cf + 128], rhs=x[:, k, s0:s0 + n],
                        start=(k == 0), stop=(k == kc - 1)),
                        reads=(w_t, x_t), writes=(ps_t,), sig=(k == kc - 1))
                cb(f0 + f, s0, n, kind, ps_[:, 0:n], ps_t)

    def proj_residual(self, W, W_ts, y, y_t, h_d, h_t, hout_d, hout_t, b0, tb, sls, gate_m, kc=KC):
        p = self.p
        state = {}

        def cb(f, s0, n, kind, ps_ap, ps_t):
            if s0 == 0:
                hc, hc_t = self.hchunk()
                p.dma("sp", hc[:, :tb], h_d[:, f, b0:b0 + tb], reads=(h_t,), writes=(hc_t,))
                state["hc"] = (hc, hc_t)
            hc, hc_t = state["hc"]
            p.op("dve", lambda e: e.scalar_tensor_tensor(
                out=hc[:, s0:s0 + n], in0=ps_ap, scalar=self.modv[:, f, kind * 6 + gate_m:kind * 6 + gate_m + 1],
                in1=hc[:, s0:s0 + n], op0=ALU.mult, op1=ALU.add),
                reads=(ps_t, hc_t, self.modv_t), writes=(hc_t,))
            if s0 + n == tb:
                p.dma("sp", hout_d[:, f, b0:b0 + tb], hc[:, :tb], reads=(hc_t,), writes=(hout_t,), join=True)
        self.proj_fm(W, W_ts, KC, y, y_t, sls, cb, kc=kc)

    def router_setup(self):
        p = self.p
        self.wr_d = p.dram_in("w_r", [128, KC, 36])
        self.br_d = p.dram_in("b_r", [128, 36])
        self.wr, self.wr_t = p.sb([128, KC, 36]), Tok()
        self.br, self.br_t = p.sb([128, 36]), Tok()
        p.dma("sp", self.wr[:], self.wr_d, writes=(self.wr_t,))
        p.dma("sp", self.br[:], self.br_d, writes=(self.br_t,))
        self.rt = [(p.sb([128, 128]), Tok()) for _ in range(2)]
        self.rt_i = 0

    def router(self, mxf, mxf_ts, c0, comb_d, row0):
        p = self.p
        ps_, ps_t = self.bank()
        for k in range(KC):
            p.op("pe", lambda e, k=k: e.matmul(ps_[:, 0:36], lhsT=mxf[:, k, c0:c0 + 128], rhs=self.wr[:, k, :],
                                               start=(k == 0), stop=(k == KC - 1)),
                 reads=tuple(mxf_ts[0:3]) + (self.wr_t,), writes=(ps_t,), sig=(k == KC - 1))
        R, R_t = self.rt[self.rt_i]
        self.rt_i ^= 1
        lg = R[:, 0:36]
        gmax, ngmax, gsum, grw = R[:, 36:37], R[:, 37:38], R[:, 38:39], R[:, 39:40]
        ge, oh, ohw = R[:, 40:44], R[:, 44:48], R[:, 48:52]
        sel, mask1, sel2, mask2 = R[:, 52:60], R[:, 60:68], R[:, 68:76], R[:, 76:84]
        m1, m2, dd, e2, w1, w2 = R[:, 84:85], R[:, 85:86], R[:, 86:87], R[:, 87:88], R[:, 88:89], R[:, 89:90]
        ing = R[:, 90:98]
        C, C_t = self.tmp()
        comb = C[:, 0:32]
        rw = (R_t,)

        def dv(fn, reads=rw, writes=rw, eng="dve"):
            p.op(eng, fn, reads=reads, writes=writes)
        dv(lambda e: e.tensor_tensor(out=lg, in0=ps_[:, 0:36], in1=self.br[:], op=ALU.add), reads=(ps_t, self.br_t))
        dv(lambda e: e.tensor_reduce(out=gmax, in_=lg[:, 0:4], axis=AX.X, op=ALU.max))
        dv(lambda e: e.tensor_scalar(out=ngmax, in0=gmax, scalar1=-1.0, scalar2=None, op0=ALU.mult))
        dv(lambda e: e.activation(out=ge, in_=lg[:, 0:4], func=AF.Exp, bias=ngmax, scale=1.0), eng="act")
        dv(lambda e: e.tensor_reduce(out=gsum, in_=ge, axis=AX.X, op=ALU.add))
        dv(lambda e: e.reciprocal(out=grw, in_=gsum))
        dv(lambda e: e.tensor_scalar(out=oh, in0=lg[:, 0:4], scalar1=gmax, scalar2=None, op0=ALU.is_equal))
        dv(lambda e: e.tensor_scalar(out=ohw, in0=oh, scalar1=grw, scalar2=None, op0=ALU.mult))
        dv(lambda e: e.tensor_scalar(out=sel, in0=lg[:, 4:12], scalar1=oh[:, 0:1], scalar2=None, op0=ALU.mult))
        for g in range(1, 4):
            dv(lambda e, g=g: e.scalar_tensor_tensor(out=sel, in0=lg[:, 4 + 8 * g:12 + 8 * g], scalar=oh[:, g:g + 1],
                                                     in1=sel, op0=ALU.mult, op1=ALU.add))
        dv(lambda e: e.tensor_reduce(out=m1, in_=sel, axis=AX.X, op=ALU.max))
        dv(lambda e: e.tensor_scalar(out=mask1, in0=sel, scalar1=m1, scalar2=None, op0=ALU.is_equal))
        dv(lambda e: e.scalar_tensor_tensor(out=sel2, in0=mask1, scalar=-1e30, in1=sel, op0=ALU.mult, op1=ALU.add))
        dv(lambda e: e.tensor_reduce(out=m2, in_=sel2, axis=AX.X, op=ALU.max))
        dv(lambda e: e.tensor_scalar(out=mask2, in0=sel2, scalar1=m2, scalar2=None, op0=ALU.is_equal))
        dv(lambda e: e.tensor_tensor(out=dd, in0=m2, in1=m1, op=ALU.subtract))
        dv(lambda e: e.activation(out=e2, in_=dd, func=AF.Exp), eng="act")
        dv(lambda e: e.tensor_scalar(out=w1, in0=e2, scalar1=1.0, scalar2=None, op0=ALU.add))
        dv(lambda e: e.reciprocal(out=w1, in_=w1))
        dv(lambda e: e.tensor_tensor(out=w2, in0=e2, in1=w1, op=ALU.mult))
        dv(lambda e: e.tensor_scalar(out=ing, in0=mask1, scalar1=w1, scalar2=None, op0=ALU.mult))
        dv(lambda e: e.scalar_tensor_tensor(out=ing, in0=mask2, scalar=w2, in1=ing, op0=ALU.mult, op1=ALU.add))
        for g in range(4):
            p.op("dve", lambda e, g=g: e.tensor_scalar(out=comb[:, 8 * g:8 * g + 8], in0=ing, scalar1=ohw[:, g:g + 1],
                                                       scalar2=None, op0=ALU.mult), reads=rw, writes=(C_t,) if g == 0 else (C_t,))
        p.out_dma("sp", comb_d[row0:row0 + 128, :], comb, reads=(C_t,))

    def norm2_router(self, h_d, h_t, b0, tb, sls, mxf, mxf_ts, mxb, mxb_t, mx_d, comb_d):
        p = self.p
        self.rms(h_d, h_t, b0, tb)
        for k in range(KC):
            hc, hc_t = self.hchunk()
            p.dma("sp", hc[:, :tb], h_d[:, k, b0:b0 + tb], reads=(h_t,), writes=(hc_t,))
            p.op("dve", lambda e, hc=hc: e.tensor_tensor(out=hc[:, :tb], in0=hc[:, :tb], in1=self.rstd[:, :tb], op=ALU.mult),
                 reads=(hc_t, self.rstd_t), writes=(hc_t,))
            for (s0, n, kind) in sls:
                p.op("act", lambda e, hc=hc, s0=s0, n=n, k=k, kind=kind: e.activation(
                    out=mxf[:, k, s0:s0 + n], in_=hc[:, s0:s0 + n], func=AF.Identity,
                    scale=self.gs[:, k, kind * 2 + 1:kind * 2 + 2], bias=self.modv[:, k, kind * 6 + 3:kind * 6 + 4]),
                    reads=(hc_t, self.gs_t, self.modv_t), writes=tuple(mxf_ts[0:3]))
            p.op("pool", lambda e, k=k: e.tensor_copy(out=mxb[:, k, :tb], in_=mxf[:, k, :tb]),
                 reads=tuple(mxf_ts[0:3]), writes=(mxb_t,))
        p.out_dma("sp", mx_d[:, :, b0:b0 + tb], mxb[:, :, :tb], reads=(mxb_t,))
        for jt in range(tb // 128):
            self.router(mxf, mxf_ts, jt * 128, comb_d, b0 + jt * 128)


def build_gmlp(T, n_part, stage=99):
    p = Prog()
    tp = TP(p, T, n_part)
    w_in = p.dram_in("w_in", [D, 2 * D])
    w_out = p.dram_in("w_out", [D, D])
    vgain_d = p.dram_in("vgain", [128, KC])
    wsT_d = p.dram_in("wsT", [128, 16, 128])
    bsb_d = p.dram_in("bsb", [128, 16, 128])
    g2_d = p.dram_in("g2prev", [128, KC, 2]) if n_part else None
    h1_d = p.dram_out("h1", [128, KC, T])
    mx_d = p.dram_out("mx", [128, KC, T], BF16)
    comb_d = p.dram_out("comb", [T, 32])
    h1_t = Tok()
    tp.router_setup()
    TB = tp.TB
    vgain, vgain_t = p.sb([128, KC]), Tok()
    wsT, wsT_t = p.sb([128, 16, 128], BF16), Tok()
    bsb, bsb_t = p.sb([128, 16, 128]), Tok()
    p.dma("sp", vgain[:], vgain_d, writes=(vgain_t,))
    p.dma("pool", wsT[:], wsT_d, writes=(wsT_t,))
    p.dma("sp", bsb[:], bsb_d, writes=(bsb_t,))
    W = p.sb([128, KC, 2048], BF16)
    W_ts = [Tok() for _ in range(4)]
    mxf = W[:].bitcast(F32)
    ax, ax_t = p.sb([128, KC, TB], BF16), Tok()
    uT, uT_t = p.sb([128, KC, TB], BF16), Tok()
    vt, vt_t = p.sb([128, 2048]), Tok()
    vsq, vsq_t = p.sb([128, 2048]), Tok()
    vh, vh_t = p.sb([128, 2048], BF16), Tok()
    sm, sm_t = p.sb([128, 4]), Tok()
    hcur, hcur_t = tp.prologue(g2_d)
    for (b0, tb, sls) in tp.blocks:
        tp.norm_mod(hcur, hcur_t, b0, tb, sls, 0, ax, ax_t)
        if stage == 1:
            p.out_dma("sp", mx_d[:, :, b0:b0 + tb], ax[:, :, :tb], reads=(ax_t,))
            continue
        tp.load_w(w_in, 0, 2048, W, W_ts)
        tp.proj_fm(W, W_ts, KC, ax, ax_t, sls,
                   lambda f, s0, n, kind, ps_ap, ps_t: tp.gelu(ps_ap, ps_t, uT[:, f, s0:s0 + n], uT_t, n))
        if stage == 2:
            p.out_dma("sp", mx_d[:, :, b0:b0 + tb], uT[:, :, :tb], reads=(uT_t,))
            continue
        tp.load_w(w_in, 2048, 2048, W, W_ts)
        for jt in range(tb // 128):
            c0 = jt * 128
            for nsl in range(4):
                ps_, ps_t = tp.bank()
                for k in range(KC):
                    p.op("pe", lambda e, k=k, nsl=nsl, c0=c0, ps_=ps_: e.matmul(
                        ps_[:, :], lhsT=ax[:, k, c0:c0 + 128], rhs=W[:, k, nsl * 512:(nsl + 1) * 512],
                        start=(k == 0), stop=(k == KC - 1)),
                        reads=(ax_t, W_ts[nsl]), writes=(ps_t,), sig=(k == KC - 1))
                tp.gelu(ps_[:, :], ps_t, vt[:, nsl * 512:(nsl + 1) * 512], vt_t, 512)
            p.op("act", lambda e: e.activation(out=vsq[:], in_=vt[:], func=AF.Square), reads=(vt_t,), writes=(vsq_t,))
            p.op("dve", lambda e: e.tensor_reduce(out=sm[:, 0:1], in_=vsq[:], axis=AX.X, op=ALU.add),
                 reads=(vsq_t,), writes=(sm_t,))
            p.op("act", lambda e: e.activation(out=sm[:, 1:2], in_=sm[:, 0:1], func=AF.Sqrt, scale=1.0 / D,
                                               bias=tp.eps_c[:, 0:1]), reads=(sm_t, tp.eps_t), writes=(sm_t,))
            p.op("dve", lambda e: e.reciprocal(out=sm[:, 2:3], in_=sm[:, 1:2]), reads=(sm_t,), writes=(sm_t,))
            p.op("dve", lambda e: e.tensor_scalar(out=vh[:], in0=vt[:], scalar1=sm[:, 2:3], scalar2=None, op0=ALU.mult),
                 reads=(sm_t, vt_t), writes=(vh_t,))
            for g4 in range(4):
                ps_, ps_t = tp.bank()
                for gi in range(4):
                    g = g4 * 4 + gi
                    p.op("pe", lambda e, g=g, gi=gi, ps_=ps_: e.matmul(
                        ps_[:, gi * 128:(gi + 1) * 128], lhsT=vh[:, g * 128:(g + 1) * 128], rhs=wsT[:, g, :],
                        start=True, stop=True), reads=(vh_t, wsT_t), writes=(ps_t,), sig=(gi == 3))
                t1, t1_t = tp.tmp()
                for gi in range(4):
                    g = g4 * 4 + gi
                    p.op("dve", lambda e, g=g, gi=gi, ps_=ps_, t1=t1: e.scalar_tensor_tensor(
                        out=t1[:, gi * 128:(gi + 1) * 128], in0=ps_[:, gi * 128:(gi + 1) * 128], scalar=vgain[:, g:g + 1],
                        in1=bsb[:, g, :], op0=ALU.mult, op1=ALU.add), reads=(ps_t, vgain_t, bsb_t), writes=(t1_t,))
                    p.op("dve", lambda e, g=g, gi=gi, t1=t1, c0=c0: e.tensor_tensor(
                        out=uT[:, g, c0:c0 + 128], in0=t1[:, gi * 128:(gi + 1) * 128], in1=uT[:, g, c0:c0 + 128], op=ALU.mult),
                        reads=(t1_t, uT_t), writes=(uT_t,))
        if stage == 3:
            p.out_dma("sp", mx_d[:, :, b0:b0 + tb], uT[:, :, :tb], reads=(uT_t,))
            continue
        tp.load_w(w_out, 0, 2048, W, W_ts)
        tp.proj_residual(W, W_ts, uT, uT_t, hcur, hcur_t, h1_d, h1_t, b0, tb, sls, 2)
        if stage == 4:
            continue
        tp.norm2_router(h1_d, h1_t, b0, tb, sls, mxf, W_ts, ax, ax_t, mx_d, comb_d)
    for t in (h1_t,):
        p.out_toks.append(t)
    return p


def fm(x):
    T = x.shape[0]
    return np.ascontiguousarray(x.T.reshape(KC, 128, T).transpose(1, 0, 2))


def unfm(a):
    T = a.shape[2]
    return np.ascontiguousarray(a.transpose(1, 0, 2).reshape(KC * 128, T).T)


def vfm(v):
    v = np.asarray(v)
    lead = v.shape[:-1]
    r = v.reshape(lead + (KC, 128))
    r = np.moveaxis(r, (-1, -2), (0, 1))
    return np.ascontiguousarray(r)


FE = 768
FC = 6


def build_moe(TA, n_exp=4):
    p = Prog()
    nb = TA // 512
    mx_d = p.dram_in("mx", [128, KC, TA], BF16)
    comb_d = p.dram_in("combT", [n_exp, TA])
    wg_d = p.dram_in("wg", [n_exp, D, FE])
    wu_d = p.dram_in("wu", [n_exp, D, FE])
    wd_d = p.dram_in("wd", [n_exp, FE, D])
    part_d = p.dram_out("part", [128, KC, TA])
    h1s = p.dram("h1s", [n_exp, 128, FC, TA], BF16)
    h1s_t = Tok()
    WB = p.sb([128, 4 * FC, 2048], BF16)
    XB = p.sb([128, 48, 512], BF16)
    pss = [(p.ps(), Tok()) for _ in range(8)]
    tmps = [(p.sb([128, 512]), Tok()) for _ in range(6)]
    cbs = [(p.sb([128, 512]), Tok()) for _ in range(2)]
    h1b = [(p.sb([128, FC, 512], BF16), Tok()) for _ in range(2)]
    st = {"ps": 0, "tmp": 0}

    def bank():
        r = pss[st["ps"]]
        st["ps"] = (st["ps"] + 1) % 8
        return r

    def tmp():
        r = tmps[st["tmp"]]
        st["tmp"] = (st["tmp"] + 1) % 6
        return r

    def wview(s, which):
        a = WB[:, s * 12 + which * 6:s * 12 + which * 6 + 6, :]
        return a.rearrange("p a b -> p (a b)").rearrange("p (k n) -> p k n", n=FE)

    w_ts = [[[Tok() for _ in range(4)] for _ in range(2)] for _ in range(2)]
    x_ts = [Tok() for _ in range(3)]
    for e in range(n_exp):
        s = e % 2
        for which, wsrc in ((0, wg_d), (1, wu_d)):
            wv = wsrc[e].rearrange("(k p) n -> p k n", p=128)
            dst = wview(s, which)
            for kk in range(0, KC, 4):
                p.dma("pool", dst[:, kk:kk + 4, :], wv[:, kk:kk + 4, :], writes=(w_ts[s][which][kk // 4],))
        Wg, Wu = wview(s, 0), wview(s, 1)
        for b in range(nb):
            b0 = b * 512
            xi = (e * nb + b) % 3
            X, X_t = XB[:, 16 * xi:16 * xi + 16, :], x_ts[xi]
            p.dma("sp", X, mx_d[:, :, b0:b0 + 512], writes=(X_t,))
            cb, cb_t = cbs[(e * nb + b) % 2]
            p.dma("sp", cb[:], comb_d[e, b0:b0 + 512].partition_broadcast(128), writes=(cb_t,))
            hb, hb_t = h1b[(e * nb + b) % 2]
            for f in range(FC):
                psg, psg_t = bank()
                psu, psu_t = bank()
                for k in range(KC):
                    p.op("pe", lambda en, k=k, f=f, psg=psg, Wg=Wg, X=X: en.matmul(
                        psg[:, :], lhsT=Wg[:, k, f * 128:(f + 1) * 128], rhs=X[:, k, :], start=(k == 0), stop=(k == KC - 1)),
                        reads=(w_ts[s][0][k // 4], X_t), writes=(psg_t,), sig=(k == KC - 1))
                for k in range(KC):
                    p.op("pe", lambda en, k=k, f=f, psu=psu, Wu=Wu, X=X: en.matmul(
                        psu[:, :], lhsT=Wu[:, k, f * 128:(f + 1) * 128], rhs=X[:, k, :], start=(k == 0), stop=(k == KC - 1)),
                        reads=(w_ts[s][1][k // 4], X_t), writes=(psu_t,), sig=(k == KC - 1))
                t1, t1_t = tmp()
                t2, t2_t = tmp()
                p.op("act", lambda en, t1=t1, psg=psg: en.activation(out=t1[:], in_=psg[:, :], func=AF.Silu),
                     reads=(psg_t,), writes=(t1_t,))
                p.op("dve", lambda en, t2=t2, psu=psu, cb=cb: en.tensor_tensor(out=t2[:], in0=psu[:, :], in1=cb[:], op=ALU.mult),
                     reads=(psu_t, cb_t), writes=(t2_t,))
                p.op("dve" if f % 2 else "pool", lambda en, t1=t1, t2=t2, hb=hb, f=f: en.tensor_tensor(
                    out=hb[:, f, :], in0=t1[:], in1=t2[:], op=ALU.mult), reads=(t1_t, t2_t), writes=(hb_t,))
            p.dma("sp", h1s[e, :, :, b0:b0 + 512], hb[:], reads=(hb_t,), writes=(h1s_t,), join=True)
    all_w = [t for a in w_ts for b_ in a for t in b_]
    wd_ts = [Tok() for _ in range(n_exp * FC)]
    for e in range(n_exp):
        wv = wd_d[e].rearrange("(c p) n -> p c n", p=128)
        for c2 in range(0, FC, 2):
            j = e * FC + c2
            p.dma("pool", WB[:, j:j + 2, :], wv[:, c2:c2 + 2, :], writes=(wd_ts[j], wd_ts[j + 1]) + tuple(all_w))
    h_ts = [Tok() for _ in range(2)]
    NJ = n_exp * FC
    for b in range(nb):
        b0 = b * 512
        H, H_t = XB[:, 24 * (b % 2):24 * (b % 2) + NJ, :], h_ts[b % 2]
        for e in range(n_exp):
            p.dma("sp", H[:, e * FC:(e + 1) * FC, :], h1s[e, :, :, b0:b0 + 512], reads=(h1s_t,),
                  writes=(H_t,) + (tuple(x_ts) if b < 2 else ()), join=(e > 0))
        for d in range(KC):
            ps_, ps_t = bank()
            for j in range(NJ):
                p.op("pe", lambda en, j=j, d=d, ps_=ps_, H=H: en.matmul(
                    ps_[:, :], lhsT=WB[:, j, d * 128:(d + 1) * 128], rhs=H[:, j, :], start=(j == 0), stop=(j == NJ - 1)),
                    reads=(wd_ts[j], H_t), writes=(ps_t,), sig=(j == NJ - 1))
            t1, t1_t = tmp()
            p.op("act" if d % 2 else "dve", (lambda en, t1=t1, ps_=ps_: en.activation(out=t1[:], in_=ps_[:, :], func=AF.Identity))
                 if d % 2 else (lambda en, t1=t1, ps_=ps_: en.tensor_copy(out=t1[:], in_=ps_[:, :])),
                 reads=(ps_t,), writes=(t1_t,))
            p.out_dma("sp", part_d[:, d, b0:b0 + 512], t1[:], reads=(t1_t,))
    return p


ATT = {
    "gqa": dict(M=1, dh=128, nq=16, nk=8, nv=1024, wcols=4096),
    "diff": dict(M=2, dh=64, nq=16, nk=16, nv=2048, wcols=6144),
}


def build_attn_a(kind, n_part):
    cfg = ATT[kind]
    T = 1152
    p = Prog()
    tp = TP(p, T, n_part)
    nq, nk, nv, dh = cfg["nq"], cfg["nk"], cfg["nv"], cfg["dh"]
    w_d = p.dram_in("w_qkv", [D, cfg["wcols"]])
    gains_d = p.dram_in("qk_gain", [128, 2])
    onesn_d = p.dram_in("onesn", [128, 128])
    rot_d = p.dram_in("rotm", [128, 128])
    cos_d = p.dram_in("cosT", [128, 1024])
    sin_d = p.dram_in("sinT", [128, 1024])
    g2_d = p.dram_in("g2prev", [128, KC, 2]) if n_part else None
    qT_d = p.dram_out("qT", [128, nq, T], BF16)
    kT_d = p.dram_out("kT", [128, nk, T], BF16)
    v_d = p.dram_out("v", [T, nv], BF16)
    h0_d = p.dram_out("h0", [128, KC, T]) if n_part else None
    TB = tp.TB
    gains, gains_t = p.sb([128, 2]), Tok()
    onesn, onesn_t = p.sb([128, 128], BF16), Tok()
    rot, rot_t = p.sb([128, 128]), Tok()
    cosT, cos_t = p.sb([128, 1024]), Tok()
    sinT, sin_t = p.sb([128, 1024]), Tok()
    p.dma("sp", gains[:], gains_d, writes=(gains_t,))
    p.dma("pool", onesn[:], onesn_d, writes=(onesn_t,))
    p.dma("sp", rot[:], rot_d, writes=(rot_t,))
    p.dma("sp", cosT[:], cos_d, writes=(cos_t,))
    p.dma("sp", sinT[:], sin_d, writes=(sin_t,))
    W = p.sb([128, KC, 2048], BF16)
    W_ts = [Tok() for _ in range(4)]
    ax, ax_t = p.sb([128, KC, TB], BF16), Tok()
    ob = [(p.sb([128, 512], BF16), Tok()) for _ in range(3)]
    vb = [(p.sb([128, 512], BF16), Tok()) for _ in range(3)]
    qn = [(p.sb([128, 512]), Tok()) for _ in range(2)]
    st = {"o": 0, "v": 0, "q": 0}
    hcur, hcur_t = tp.prologue(g2_d)
    if n_part:
        for k in range(KC):
            hc, hc_t = tp.hchunk()
            for (b0, tb, sls) in tp.blocks:
                p.dma("sp", hc[:, :tb], hcur[:, k, b0:b0 + tb], reads=(hcur_t,), writes=(hc_t,))
                p.out_dma("sp", h0_d[:, k, b0:b0 + tb], hc[:, :tb], reads=(hc_t,))

    def qk_cb(which, out_d, fbase):
        def cb(f, s0, n, kd, ps_ap, ps_t):
            b0 = cb.b0
            sq, sq_t = tp.tmp()
            sqb = sq[:].bitcast(BF16)[:, 0:n]
            p.op("act", lambda e: e.activation(out=sqb, in_=ps_ap, func=AF.Square), reads=(ps_t,), writes=(sq_t,))
            ps2, ps2_t = tp.bank()
            p.op("pe", lambda e: e.matmul(ps2[:, 0:n], lhsT=onesn[:], rhs=sqb, start=True, stop=True),
                 reads=(sq_t, onesn_t), writes=(ps2_t,))
            r, r_t = tp.tmp()
            p.op("act", lambda e: e.activation(out=r[:, :n], in_=ps2[:, 0:n], func=AF.Sqrt, scale=1.0 / dh,
                                               bias=tp.eps_c[:, 0:1]), reads=(ps2_t, tp.eps_t), writes=(r_t,))
            p.op("dve", lambda e: e.reciprocal(out=r[:, :n], in_=r[:, :n]), reads=(r_t,), writes=(r_t,))
            q_, q_t = qn[st["q"]]
            st["q"] ^= 1
            p.op("dve", lambda e: e.scalar_tensor_tensor(out=q_[:, :n], in0=ps_ap, scalar=gains[:, which:which + 1],
                                                         in1=r[:, :n], op0=ALU.mult, op1=ALU.mult),
                 reads=(ps_t, r_t, gains_t), writes=(q_t,))
            o_, o_t = ob[st["o"]]
            st["o"] = (st["o"] + 1) % 3
            if kd == 0:
                c0 = b0 + s0
                ps3, ps3_t = tp.bank()
                p.op("pe", lambda e: e.matmul(ps3[:, 0:n], lhsT=rot[:], rhs=q_[:, :n], start=True, stop=True),
                     reads=(q_t, rot_t), writes=(ps3_t,))
                t2, t2_t = tp.tmp()
                p.op("dve", lambda e: e.tensor_tensor(out=t2[:, :n], in0=ps3[:, 0:n], in1=sinT[:, c0:c0 + n], op=ALU.mult),
                     reads=(ps3_t, sin_t), writes=(t2_t,))
                p.op("pool", lambda e: e.tensor_tensor(out=q_[:, :n], in0=q_[:, :n], in1=cosT[:, c0:c0 + n], op=ALU.mult),
                     reads=(q_t, cos_t), writes=(q_t,))
                p.op("dve", lambda e: e.tensor_tensor(out=o_[:, :n], in0=q_[:, :n], in1=t2[:, :n], op=ALU.add),
                     reads=(q_t, t2_t), writes=(o_t,))
            else:
                p.op("dve", lambda e: e.tensor_copy(out=o_[:, :n], in_=q_[:, :n]), reads=(q_t,), writes=(o_t,))
            p.out_dma("sp", out_d[:, f - fbase, b0 + s0:b0 + s0 + n], o_[:, :n], reads=(o_t,))
        return cb

    for (b0, tb, sls) in tp.blocks:
        tp.norm_mod(hcur, hcur_t, b0, tb, sls, 0, ax, ax_t)
        for wbase in range(0, cfg["wcols"], 2048):
            tp.load_w(w_d, wbase, 2048, W, W_ts)
            cbase = wbase // 128
            for (which, lo, hi, out_d) in ((0, 0, nq, qT_d), (1, nq, nq + nk, kT_d)):
                a, b_ = max(lo, cbase), min(hi, cbase + 16)
                if a < b_:
                    cb = qk_cb(which, out_d, lo)
                    cb.b0 = b0
                    tp.proj_fm(W, W_ts, b_ - a, ax, ax_t, sls, cb, f0=a, col0=(a - cbase) * 128)
            va, vb_ = max((nq + nk) * 128, wbase), min(cfg["wcols"], wbase + 2048)
            if va < vb_:
                for jt in range(tb // 128):
                    c0 = jt * 128
                    for cc in range(va, vb_, 512):
                        ps_, ps_t = tp.bank()
                        for k in range(KC):
                            p.op("pe", lambda e, k=k, c0=c0, cc=cc, ps_=ps_: e.matmul(
                                ps_[:, :], lhsT=ax[:, k, c0:c0 + 128], rhs=W[:, k, cc - wbase:cc - wbase + 512],
                                start=(k == 0), stop=(k == KC - 1)),
                                reads=(ax_t, W_ts[(cc - wbase) // 512]), writes=(ps_t,), sig=(k == KC - 1))
                        v_, v_t = vb[st["v"]]
                        st["v"] = (st["v"] + 1) % 3
                        p.op("act", lambda e, v_=v_, ps_=ps_: e.activation(out=v_[:], in_=ps_[:, :], func=AF.Identity),
                             reads=(ps_t,), writes=(v_t,))
                        vc = cc - (nq + nk) * 128
                        p.out_dma("sp", v_d[b0 + c0:b0 + c0 + 128, vc:vc + 512], v_[:], reads=(v_t,))
    return p


def build_attn_b(kind, T):
    cfg = ATT[kind]
    M, dh, nq, nk, nv = cfg["M"], cfg["dh"], cfg["nq"], cfg["nk"], cfg["nv"]
    TK = 2304
    NT = TK // 128
    CTX_TILES = (8, 17)
    p = Prog()
    tp = TP(p, T, 0)
    qT_d = p.dram_in("qT", [128, nq, T], BF16)
    kT_d = p.dram_in("kT", [128, nk, TK], BF16)
    v_d = p.dram_in("v", [TK, nv], BF16)
    wo_d = p.dram_in("w_o", [D, D])
    h1_d = p.dram_out("h1", [128, KC, T])
    mx_d = p.dram_out("mx", [128, KC, T], BF16)
    comb_d = p.dram_out("comb", [T, 32])
    h1_t = Tok()
    tp.router_setup()
    TB = tp.TB
    if kind == "diff":
        lam_d = p.dram_in("lamv", [128, 4, 64])
        subg_d = p.dram_in("subg", [128, 2])
        lamv, lamv_t = p.sb([128, 4, 64]), Tok()
        subg, subg_t = p.sb([128, 2]), Tok()
        lam, lam_t = p.sb([128, 8]), Tok()
        p.dma("sp", lamv[:], lam_d, writes=(lamv_t,))
        p.dma("sp", subg[:], subg_d, writes=(subg_t,))
        lt = (lam_t,)
        p.op("dve", lambda e: e.tensor_tensor(out=lamv[:, 0, :], in0=lamv[:, 0, :], in1=lamv[:, 1, :], op=ALU.mult),
             reads=(lamv_t,), writes=(lamv_t,))
        p.op("dve", lambda e: e.tensor_tensor(out=lamv[:, 2, :], in0=lamv[:, 2, :], in1=lamv[:, 3, :], op=ALU.mult),
             reads=(lamv_t,), writes=(lamv_t,))
        p.op("dve", lambda e: e.tensor_reduce(out=lam[:, 0:1], in_=lamv[:, 0, :], axis=AX.X, op=ALU.add), reads=(lamv_t,), writes=lt)
        p.op("dve", lambda e: e.tensor_reduce(out=lam[:, 1:2], in_=lamv[:, 2, :], axis=AX.X, op=ALU.add), reads=(lamv_t,) + lt, writes=lt)
        p.op("act", lambda e: e.activation(out=lam[:, 2:4], in_=lam[:, 0:2], func=AF.Exp), reads=lt, writes=lt)
        p.op("dve", lambda e: e.tensor_tensor(out=lam[:, 4:5], in0=lam[:, 3:4], in1=lam[:, 2:3], op=ALU.subtract), reads=lt, writes=lt)
        p.op("dve", lambda e: e.tensor_tensor(out=lam[:, 5:6], in0=lam[:, 4:5], in1=subg[:, 1:2], op=ALU.subtract),
             reads=lt + (subg_t,), writes=lt)
        p.op("dve", lambda e: e.tensor_scalar(out=lam[:, 6:7], in0=subg[:, 1:2], scalar1=-1.0, scalar2=1.0, op0=ALU.mult, op1=ALU.add),
             reads=lt + (subg_t,), writes=lt)
        p.op("dve", lambda e: e.tensor_tensor(out=lam[:, 6:7], in0=lam[:, 6:7], in1=subg[:, 0:1], op=ALU.mult),
             reads=lt + (subg_t,), writes=lt)
    W = p.sb([128, KC, 2048], BF16)
    W_ts = [Tok() for _ in range(4)]
    mxf = W[:].bitcast(F32)
    oT, oT_t = p.sb([128, KC, TB], BF16), Tok()
    mxb, mxb_t = p.sb([128, KC, TB], BF16), Tok()
    kb = [(p.sb([128, TK], BF16), Tok()) for _ in range(2)]
    vbuf = [(p.sb([128, NT, 128], BF16), Tok()) for _ in range(2)]
    qb = [(p.sb([128, TB], BF16), Tok()) for _ in range(2)]
    pb = [(p.sb([128, 512], BF16), Tok()) for _ in range(4)]
    accs = tp.pss[0:4]
    sbanks = tp.pss[4:8]
    st = {"s": 0, "p": 0}

    def sbank():
        r = sbanks[st["s"]]
        st["s"] = (st["s"] + 1) % 4
        return r
    tp.pss = sbanks
    tp.ps_i = 0
    scale = 1.0 / float(np.sqrt(dh))
    for bi, (b0, tb, sls) in enumerate(tp.blocks):
        for h in range(nq):
            hi = (bi * nq + h) % 2
            kh = h // (nq // nk)
            vh = h if nv == 2048 else h // 2
            K_, K_t = kb[hi]
            V_, V_t = vbuf[hi]
            Q_, Q_t = qb[hi]
            p.dma("sp", K_[:], kT_d[:, kh, :], writes=(K_t,))
            p.dma("sp", V_[:], v_d[:, vh * 128:(vh + 1) * 128].rearrange("(j t) d -> t j d", t=128), writes=(V_t,))
            p.dma("sp", Q_[:, :tb], qT_d[:, h, b0:b0 + tb], writes=(Q_t,))
            for (s0, n, kd) in sls:
                tiles = list(range(NT)) if kd == 0 else list(CTX_TILES)
                for ji, j in enumerate(tiles):
                    first, last = ji == 0, ji == len(tiles) - 1
                    for m in range(M):
                        pm = slice(m * 64, m * 64 + 64) if M == 2 else slice(0, 128)
                        S_, S_t = sbank()
                        p.op("pe", lambda e, S_=S_, pm=pm, j=j, K_=K_, Q_=Q_, s0=s0, n=n: e.matmul(
                            S_[:, 0:n], lhsT=K_[pm, j * 128:(j + 1) * 128], rhs=Q_[pm, s0:s0 + n], start=True, stop=True),
                            reads=(K_t, Q_t), writes=(S_t,))
                        P_, P_t = pb[st["p"]]
                        st["p"] = (st["p"] + 1) % 4
                        p.op("act", lambda e, S_=S_, P_=P_, n=n: e.activation(out=P_[:, :n], in_=S_[:, 0:n], func=AF.Exp, scale=scale),
                             reads=(S_t,), writes=(P_t,))
                        O_, O_t = accs[2 * m]
                        Dn, Dn_t = accs[2 * m + 1]
                        p.op("pe", lambda e, O_=O_, V_=V_, P_=P_, j=j, n=n, first=first, last=last: e.matmul(
                            O_[:, 0:n], lhsT=V_[:, j, :], rhs=P_[:, :n], start=first, stop=last),
                            reads=(V_t, P_t), writes=(O_t,))
                        p.op("pe", lambda e, Dn=Dn, P_=P_, n=n, first=first, last=last: e.matmul(
                            Dn[:, 0:n], lhsT=tp.ones_bf[:], rhs=P_[:, :n], start=first, stop=last),
                            reads=(tp.ones_t, P_t), writes=(Dn_t,))
                outs = []
                for m in range(M):
                    O_, O_t = accs[2 * m]
                    Dn, Dn_t = accs[2 * m + 1]
                    r, r_t = tp.tmp()
                    p.op("dve", lambda e, r=r, Dn=Dn, n=n: e.reciprocal(out=r[:, :n], in_=Dn[:, 0:n]), reads=(Dn_t,), writes=(r_t,))
                    if M == 1:
                        p.op("dve", lambda e, r=r, O_=O_, n=n, h=h, s0=s0: e.tensor_tensor(
                            out=oT[:, h, s0:s0 + n], in0=O_[:, 0:n], in1=r[:, :n], op=ALU.mult),
                            reads=(O_t, r_t), writes=(oT_t,))
                    else:
                        p.op("dve", lambda e, r=r, O_=O_, n=n: e.tensor_tensor(out=r[:, :n], in0=O_[:, 0:n], in1=r[:, :n], op=ALU.mult),
                             reads=(O_t, r_t), writes=(r_t,))
                        outs.append((r, r_t))
                if M == 2:
                    (o0, o0_t), (o1, o1_t) = outs
                    p.op("dve", lambda e, o0=o0, o1=o1, n=n: e.scalar_tensor_tensor(
                        out=o0[:, :n], in0=o1[:, :n], scalar=lam[:, 5:6], in1=o0[:, :n], op0=ALU.mult, op1=ALU.add),
                        reads=(o0_t, o1_t, lam_t), writes=(o0_t,))
                    sq, sq_t = tp.tmp()
                    sqb = sq[:].bitcast(BF16)[:, 0:n]
                    p.op("act", lambda e, sqb=sqb, o0=o0, n=n: e.activation(out=sqb, in_=o0[:, :n], func=AF.Square),
                         reads=(o0_t,), writes=(sq_t,))
                    ps2, ps2_t = sbank()
                    p.op("pe", lambda e, ps2=ps2, sqb=sqb, n=n: e.matmul(ps2[:, 0:n], lhsT=tp.ones_bf[:], rhs=sqb, start=True, stop=True),
                         reads=(sq_t, tp.ones_t), writes=(ps2_t,))
                    r2, r2_t = tp.tmp()
                    p.op("act", lambda e, r2=r2, ps2=ps2, n=n: e.activation(out=r2[:, :n], in_=ps2[:, 0:n], func=AF.Sqrt, scale=1.0 / 128,
                                                                          bias=tp.eps_c[:, 0:1]), reads=(ps2_t, tp.eps_t), writes=(r2_t,))
                    p.op("dve", lambda e, r2=r2, n=n: e.reciprocal(out=r2[:, :n], in_=r2[:, :n]), reads=(r2_t,), writes=(r2_t,))
                    p.op("dve", lambda e, o0=o0, r2=r2, n=n, h=h, s0=s0: e.scalar_tensor_tensor(
                        out=oT[:, h, s0:s0 + n], in0=o0[:, :n], scalar=lam[:, 6:7], in1=r2[:, :n], op0=ALU.mult, op1=ALU.mult),
                        reads=(o0_t, r2_t, lam_t), writes=(oT_t,))
        tp.load_w(wo_d, 0, 2048, W, W_ts)
        tp.proj_residual(W, W_ts, oT, oT_t, tp.h_in, Tok(), h1_d, h1_t, b0, tb, sls, 2)
        tp.norm2_router(h1_d, h1_t, b0, tb, sls, mxf, W_ts, mxb, mxb_t, mx_d, comb_d)
    p.out_toks.append(h1_t)
    return p


def build_final(T=1024, n_part=8):
    p = Prog()
    tp = TP(p, T, n_part)
    g2_d = p.dram_in("g2prev", [128, KC, 2])
    out_d = p.dram_out("hout", [128, KC, T])
    hcur, hcur_t = tp.prologue(g2_d)
    for k in range(KC):
        for (b0, tb, sls) in tp.blocks:
            hc, hc_t = tp.hchunk()
            p.dma("sp", hc[:, :tb], hcur[:, k, b0:b0 + tb], reads=(hcur_t,), writes=(hc_t,))
            p.out_dma("sp", out_d[:, k, b0:b0 + tb], hc[:, :tb], reads=(hc_t,))
    return p


def rope_tables(dh, half_idx):
    t = np.arange(half_idx * 1024, half_idx * 1024 + 1024)
    row = (t // 64).astype(np.float32)
    col = (t % 64).astype(np.float32)
    hf = dh // 2
    inv = (10000.0 ** (-np.arange(0, hf, 2, dtype=np.float32) / hf)).astype(np.float32)
    ar = row[:, None] * inv[None, :]
    ac = col[:, None] * inv[None, :]
    ang = np.concatenate([ar, ar, ac, ac], -1)
    cos, sin = np.cos(ang).astype(np.float32), np.sin(ang).astype(np.float32)
    rep = 128 // dh
    cos, sin = np.tile(cos, (1, rep)), np.tile(sin, (1, rep))
    q = dh // 4
    rm = np.zeros((128, 128), np.float32)
    for base in range(0, 128, dh):
        for m in range(dh):
            blk = m // q
            if blk in (0, 2):
                rm[base + m + q, base + m] = -1.0
            else:
                rm[base + m - q, base + m] = 1.0
    ones = np.zeros((128, 128), np.float32)
    for base in range(0, 128, dh):
        ones[base:base + dh, base:base + dh] = 1.0
    return np.ascontiguousarray(cos.T), np.ascontiguousarray(sin.T), rm, ones


def _c(a):
    return np.ascontiguousarray(a)


def kernel(x, c, ctx, c_ctx, ada_w, ada_b, norm1_g, norm2_g,
           gmlp_w_in, gmlp_v_gain, gmlp_w_s, gmlp_b_s, gmlp_w_out,
           gqa_w_qkv, gqa_q_gain, gqa_k_gain, gqa_w_o,
           diff_w_qkv, diff_q_gain, diff_k_gain, diff_lam_q1, diff_lam_k1, diff_lam_q2, diff_lam_k2,
           diff_sub_gain, diff_w_o,
           moe_w_grp, moe_b_grp, moe_w_exp, moe_b_exp, moe_w_gate, moe_w_up, moe_w_down):
    f32 = np.float32
    A = lambda a: np.asarray(a, dtype=f32)
    x, c, ctx, c_ctx = A(x), A(c), A(ctx), A(c_ctx)
    depth = 4
    NC = NCORES
    c_all = np.concatenate([c, c_ctx[None]], 0)
    cT = _c(c_all.reshape(5, KC, 128).transpose(2, 1, 0))
    ada_w, ada_b = A(ada_w), A(ada_b)
    maps = []
    for ci in range(NC):
        sl = slice(ci * ADA_COLS, (ci + 1) * ADA_COLS)
        maps.append({"cT": cT, "w": _c(ada_w[:, :, sl]),
                     "b": _c(np.broadcast_to(ada_b[:, None, sl], (depth, 5, ADA_COLS)))})
    res = launch(build_ada(depth), maps)
    mod_full = np.concatenate([r["mod"] for r in res], axis=-1)
    del maps, res

    def modv_for(i, b):
        ml = mod_full[i][[b, 4]].reshape(12, D)
        return vfm(ml)

    def g2_for(i, b):
        return vfm(mod_full[i][[b, 4]].reshape(2, 6, D)[:, 5, :])

    def ng_for(i):
        return vfm(np.stack([A(norm1_g)[i], A(norm2_g)[i]], 0))

    def router_in(i):
        w_r = np.concatenate([A(moe_w_grp)[i], A(moe_w_exp)[i]], 1)
        b_r = np.concatenate([A(moe_b_grp)[i], A(moe_b_exp)[i]], 0)
        return {"w_r": _c(w_r.reshape(KC, 128, 36).transpose(1, 0, 2)),
                "b_r": _c(np.broadcast_to(b_r[None], (128, 36)))}

    def moe(i, mx_list, comb_list):
        TA = sum(m.shape[2] for m in mx_list)
        mx_all = _c(np.concatenate(mx_list, 2))
        comb_all = np.concatenate(comb_list, 0)
        wg = A(moe_w_gate)[i].reshape(32, D, FE)
        wu = A(moe_w_up)[i].reshape(32, D, FE)
        wd = A(moe_w_down)[i].reshape(32, FE, D)
        maps = [{"mx": mx_all, "combT": _c(comb_all[:, 4 * ci:4 * ci + 4].T),
                 "wg": _c(wg[4 * ci:4 * ci + 4]), "wu": _c(wu[4 * ci:4 * ci + 4]), "wd": _c(wd[4 * ci:4 * ci + 4])}
                for ci in range(NC)]
        res = launch(build_moe(TA), maps)
        return [r["part"] for r in res]

    def parts_for(parts, ci, T):
        return _c(np.stack([pp[:, :, ci * T:(ci + 1) * T] for pp in parts], 0))

    h_list = []
    for ci in range(NC):
        b, hf = ci // 2, ci % 2
        tok = np.concatenate([x[b, hf * 1024:(hf + 1) * 1024], ctx[b, hf * 128:(hf + 1) * 128]], 0)
        h_list.append(fm(tok))

    def gmlp_layer(i, j, T, h_list, parts):
        common = {"ng": ng_for(i), "w_in": A(gmlp_w_in)[j], "w_out": A(gmlp_w_out)[j], "vgain": vfm(A(gmlp_v_gain)[j]),
                  "wsT": _c(A(gmlp_w_s)[j].transpose(2, 0, 1)),
                  "bsb": _c(np.broadcast_to(A(gmlp_b_s)[j][None], (128, 16, 128)))}
        common.update(router_in(i))
        maps = []
        for ci in range(NC):
            m = dict(common)
            m["h_in"] = h_list[ci]
            m["modv"] = modv_for(i, ci // 2)
            if parts is not None:
                m["part"] = parts_for(parts, ci, T)
                m["g2prev"] = g2_for(i - 1, ci // 2)
            maps.append(m)
        res = launch(build_gmlp(T, 0 if parts is None else NC), maps)
        return [r["h1"] for r in res], [r["mx"] for r in res], [r["comb"] for r in res]

    def attn_layer(kind, i, h_list, parts, TQ):
        cfg = ATT[kind]
        if kind == "gqa":
            w_qkv, w_o = A(gqa_w_qkv)[0], A(gqa_w_o)[0]
            gains = np.stack([A(gqa_q_gain)[0], A(gqa_k_gain)[0]], 1)
        else:
            w_qkv, w_o = A(diff_w_qkv)[0], A(diff_w_o)[0]
            gains = np.stack([A(diff_q_gain)[0].reshape(128), A(diff_k_gain)[0].reshape(128)], 1)
        maps = []
        for ci in range(NC):
            cosT, sinT, rm, onesn = rope_tables(cfg["dh"], ci % 2)
            maps.append({"h_in": h_list[ci], "modv": modv_for(i, ci // 2), "ng": ng_for(i), "w_qkv": w_qkv,
                         "qk_gain": _c(gains), "onesn": onesn, "rotm": rm, "cosT": cosT, "sinT": sinT,
                         "part": parts_for(parts, ci, 1152), "g2prev": g2_for(i - 1, ci // 2)})
        resA = launch(build_attn_a(kind, NC), maps)
        del maps
        mapsB = []
        rin = router_in(i)
        for ci in range(NC):
            e, o = (ci // 2) * 2, (ci // 2) * 2 + 1
            m = {"h_in": _c(resA[ci]["h0"][:, :, :TQ]), "modv": modv_for(i, ci // 2), "ng": ng_for(i),
                 "qT": _c(resA[ci]["qT"][:, :, :TQ]),
                 "kT": _c(np.concatenate([resA[e]["kT"], resA[o]["kT"]], 2)),
                 "v": _c(np.concatenate([resA[e]["v"], resA[o]["v"]], 0)), "w_o": w_o}
            m.update(rin)
            if kind == "diff":
                lq = np.stack([A(diff_lam_q1)[0], A(diff_lam_k1)[0], A(diff_lam_q2)[0], A(diff_lam_k2)[0]], 0)
                lam_init = 0.8 - 0.6 * float(np.exp(-0.3 * i))
                m["lamv"] = _c(np.broadcast_to(lq[None], (128, 4, 64)))
                m["subg"] = _c(np.stack([A(diff_sub_gain)[0], np.full(128, lam_init, f32)], 1).astype(f32))
            mapsB.append(m)
        resB = launch(build_attn_b(kind, TQ), mapsB)
        h0_ctx = [r["h0"] for r in resA]
        return [r["h1"] for r in resB], [r["mx"] for r in resB], [r["comb"] for r in resB], h0_ctx

    h1, mx, comb = gmlp_layer(0, 0, 1152, h_list, None)
    parts = moe(0, mx, comb)
    h1, mx, comb, _ = attn_layer("gqa", 1, h1, parts, 1152)
    parts = moe(1, mx, comb)
    h1, mx, comb, _ = attn_layer("diff", 2, h1, parts, 1024)
    parts = moe(2, mx, comb)
    h1, mx, comb = gmlp_layer(3, 1, 1024, h1, parts)
    parts = moe(3, mx, comb)
    maps = [{"h_in": h1[ci], "modv": modv_for(3, ci // 2), "ng": ng_for(3), "part": parts_for(parts, ci, 1024),
             "g2prev": g2_for(3, ci // 2)} for ci in range(NC)]
    res = launch(build_final(1024, NC), maps)
    out = np.zeros((4, 2048, D), f32)
    for ci in range(NC):
        b, hf = ci // 2, ci % 2
        out[b, hf * 1024:(hf + 1) * 1024] = unfm(res[ci]["hout"])
    return out
```

```python
import contextlib
import numpy as np
import ml_dtypes
import concourse.bass as bass
import concourse.mybir as mybir
from concourse.bass_utils import run_bass_kernel_spmd

F32 = mybir.dt.float32
BF16 = mybir.dt.bfloat16
AF = mybir.ActivationFunctionType
ALU = mybir.AluOpType
AX = mybir.AxisListType
NCORES = 8
D = 2048
KC = 16
EPS = 1e-6


class Tok:
    __slots__ = ("w", "r")

    def __init__(self):
        self.w = []
        self.r = {}


class Prog:
    ENG = ("pe", "act", "dve", "pool", "sp")

    def __init__(self, n_dma_sems=10):
        self.nc = bass.Bass("TRN2", target_bir_lowering=False)
        self.es = contextlib.ExitStack()
        self.sems = {}
        self.cnt = {}
        self.known = {e: {} for e in self.ENG}
        self.q = {e: [] for e in self.ENG}
        for e in self.ENG:
            self._mk("e_" + e)
        self.dsem = ["d%d" % i for i in range(n_dma_sems)]
        for s in self.dsem:
            self._mk(s)
        self.dnext = 0
        self.out_toks = []
        self.n_t = 0

    def _mk(self, name):
        self.sems[name] = self.es.enter_context(self.nc.semaphore(name))
        self.cnt[name] = 0

    def dram_in(self, name, shape, dt=F32):
        return self.nc.dram_tensor(name, list(shape), dt, kind="ExternalInput").ap()

    def dram_out(self, name, shape, dt=F32):
        return self.nc.dram_tensor(name, list(shape), dt, kind="ExternalOutput").ap()

    def dram(self, name, shape, dt=F32):
        return self.nc.dram_tensor(name, list(shape), dt).ap()

    def sb(self, shape, dt=F32, name=None):
        self.n_t += 1
        return self.es.enter_context(self.nc.sbuf_tensor(name or "t%d" % self.n_t, list(shape), dt))

    def ps(self, shape=(128, 512), dt=F32, name=None):
        self.n_t += 1
        return self.es.enter_context(self.nc.psum_tensor(name or "p%d" % self.n_t, list(shape), dt))

    def _need(self, eng, waits, tok):
        if tok is None:
            return
        s, v = tok
        if eng == "pe" and s == "e_pe":
            return
        if self.known[eng].get(s, 0) < v:
            waits[s] = max(waits.get(s, 0), v)

    def _deps(self, eng, reads, writes):
        waits = {}
        for o in reads:
            for t in o.w:
                self._need(eng, waits, t)
        for o in writes:
            for t in o.w:
                self._need(eng, waits, t)
            for s, v in o.r.items():
                self._need(eng, waits, (s, v))
        for s, v in waits.items():
            self.known[eng][s] = v
        return list(waits.items())

    def op(self, eng, fn, reads=(), writes=(), sig=True):
        waits = self._deps(eng, reads, writes)
        s = "e_" + eng
        if sig:
            self.cnt[s] += 1
            tok = (s, self.cnt[s])
        else:
            tok = (s, self.cnt[s] + 1)
        for o in reads:
            o.r[tok[0]] = max(o.r.get(tok[0], 0), tok[1])
        for o in writes:
            o.w = [tok]
            o.r = {}
        self.q[eng].append((waits, fn, (s, 1) if sig else None))

    def dma(self, queue, out, in_, reads=(), writes=(), join=False, **kw):
        s = self.dsem[self.dnext]
        self.dnext = (self.dnext + 1) % len(self.dsem)
        waits = self._deps(queue, reads, writes)
        prev = self.cnt[s]
        if prev and self.known[queue].get(s, 0) < prev:
            waits.append((s, prev))
            self.known[queue][s] = prev
        self.cnt[s] += 16
        tok = (s, self.cnt[s])
        for o in reads:
            o.r[s] = tok[1]
        for o in writes:
            if join:
                o.w = [t for t in o.w if t[0] != s] + [tok]
            else:
                o.w = [tok]
                o.r = {}
        self.q[queue].append((waits, lambda e: e.dma_start(out=out, in_=in_, **kw), (s, 16)))

    def out_dma(self, queue, out, in_, reads=()):
        t = Tok()
        self.dma(queue, out, in_, reads=reads, writes=(t,))
        self.out_toks.append(t)

    def build(self):
        waits = self._deps("sp", self.out_toks, ())
        self.q["sp"].append((waits, None, None))
        nc = self.nc

        def run(e, name):
            for waits, fn, inc in self.q[name]:
                for s, v in waits:
                    e.wait_ge(self.sems[s], v)
                if fn is None:
                    continue
                ins = fn(e)
                if inc is not None:
                    ins.then_inc(self.sems[inc[0]], inc[1])

        with nc.Block() as block:
            @block.tensor
            def _(e):
                run(e, "pe")

            @block.scalar
            def _(e):
                run(e, "act")

            @block.vector
            def _(e):
                run(e, "dve")

            @block.gpsimd
            def _(e):
                run(e, "pool")

            @block.sync
            def _(e):
                run(e, "sp")
        self.es.close()
        return nc


def launch(prog, in_maps):
    nc = prog.build()
    res = run_bass_kernel_spmd(nc, in_maps, core_ids=list(range(len(in_maps))))
    return res.results


ADA_COLS = 6 * D // NCORES


def build_ada(depth):
    p = Prog()
    cT = p.dram_in("cT", [128, KC, 5])
    w = p.dram_in("w", [depth, D, ADA_COLS])
    b = p.dram_in("b", [depth, 5, ADA_COLS])
    out = p.dram_out("mod", [depth, 5, ADA_COLS])
    c_sb, c_t = p.sb([128, KC, 5]), Tok()
    s_sb, s_t = p.sb([128, KC, 5]), Tok()
    p.dma("sp", c_sb[:], cT, writes=(c_t,))
    p.op("act", lambda e: e.activation(out=s_sb[:], in_=c_sb[:], func=AF.Silu), reads=(c_t,), writes=(s_t,))
    w_sb = p.sb([128, KC, ADA_COLS])
    w_ts = [Tok() for _ in range(4)]
    bb = [(p.sb([5, ADA_COLS]), Tok()) for _ in range(2)]
    ob = [(p.sb([5, ADA_COLS]), Tok()) for _ in range(2)]
    pss = [(p.ps(), Tok()) for _ in range(3)]
    for l in range(depth):
        b_sb, b_t = bb[l % 2]
        o_sb, o_t = ob[l % 2]
        wv = w[l].rearrange("(k p) n -> p k n", p=128)
        for kk in range(0, KC, 4):
            p.dma("sp", w_sb[:, kk:kk + 4, :], wv[:, kk:kk + 4, :], writes=(w_ts[kk // 4],))
        p.dma("sp", b_sb[:], b[l], writes=(b_t,))
        for n in range(3):
            ps_, ps_t = pss[n]
            for k in range(KC):
                p.op("pe", lambda e, k=k, n=n, ps_=ps_: e.matmul(
                    ps_[0:5, :], lhsT=s_sb[:, k, :], rhs=w_sb[:, k, n * 512:(n + 1) * 512],
                    start=(k == 0), stop=(k == KC - 1)),
                    reads=(s_t, w_ts[k // 4]), writes=(ps_t,), sig=(k == KC - 1))
            p.op("dve", lambda e, n=n, ps_=ps_, o_sb=o_sb, b_sb=b_sb: e.tensor_tensor(
                out=o_sb[:, n * 512:(n + 1) * 512], in0=ps_[0:5, :], in1=b_sb[:, n * 512:(n + 1) * 512], op=ALU.add),
                reads=(ps_t, b_t), writes=(o_t,))
        p.out_dma("sp", out[l], o_sb[:], reads=(o_t,))
    return p


GELU_C = 1.5957691216057308


class TP:
    def __init__(self, p, T, n_part, n_hch=3):
        self.p = p
        self.T = T
        self.n_lat = 1024
        self.has_ctx = T > 1024
        self.n_part = n_part
        if self.has_ctx:
            self.blocks = [(0, 512, [(0, 512, 0)]), (512, 640, [(0, 512, 0), (512, 128, 1)])]
        else:
            self.blocks = [(0, 512, [(0, 512, 0)]), (512, 512, [(0, 512, 0)])]
        self.TB = 640 if self.has_ctx else 512
        self.h_in = p.dram_in("h_in", [128, KC, T])
        self.modv_d = p.dram_in("modv", [128, KC, 12])
        self.ng_d = p.dram_in("ng", [128, KC, 2])
        if n_part:
            self.part_d = p.dram_in("part", [n_part, 128, KC, T])
        self.modv, self.modv_t = p.sb([128, KC, 12]), Tok()
        self.ng, self.ng_t = p.sb([128, KC, 2]), Tok()
        self.gs, self.gs_t = p.sb([128, KC, 4]), Tok()
        p.dma("sp", self.modv[:], self.modv_d, writes=(self.modv_t,))
        p.dma("sp", self.ng[:], self.ng_d, writes=(self.ng_t,))
        for kind in range(2):
            for which in range(2):
                j = kind * 2 + which
                sc = self.modv[:, :, kind * 6 + which * 3 + 1]
                p.op("dve", lambda e, j=j, sc=sc, which=which: e.scalar_tensor_tensor(
                    out=self.gs[:, :, j], in0=sc, scalar=1.0, in1=self.ng[:, :, which], op0=ALU.add, op1=ALU.mult),
                    reads=(self.modv_t, self.ng_t), writes=(self.gs_t,))
        self.eps_c, self.eps_t = p.sb([128, 1]), Tok()
        p.op("dve", lambda e: e.memset(self.eps_c[:], EPS), writes=(self.eps_t,))
        self.ones_bf, self.ones_t = p.sb([128, 128], BF16), Tok()
        p.op("dve", lambda e: e.memset(self.ones_bf[:], 1.0), writes=(self.ones_t,))
        self.pss = [(p.ps(), Tok()) for _ in range(8)]
        self.ps_i = 0
        self.tmps = [(p.sb([128, 512]), Tok()) for _ in range(6)]
        self.tmp_i = 0
        self.hch = [(p.sb([128, self.TB]), Tok()) for _ in range(n_hch)]
        self.hch_i = 0
        self.accs = [(p.sb([128, self.TB]), Tok()) for _ in range(2 if n_hch <= 3 else 3)] if n_part else []
        self.acc_i = 0
        self.rstd, self.rstd_t = p.sb([128, self.TB]), Tok()
        self.sq = [(p.sb([128, self.TB], BF16), Tok()) for _ in range(2)]
        self.sq_i = 0

    def bank(self):
        r = self.pss[self.ps_i]
        self.ps_i = (self.ps_i + 1) % len(self.pss)
        return r

    def tmp(self):
        r = self.tmps[self.tmp_i]
        self.tmp_i = (self.tmp_i + 1) % len(self.tmps)
        return r

    def hchunk(self):
        r = self.hch[self.hch_i]
        self.hch_i = (self.hch_i + 1) % len(self.hch)
        return r

    def mod(self, kind, m):
        return self.modv[:, :, kind * 6 + m]

    def prologue(self, g2_d=None, out_d=None):
        p = self.p
        if not self.n_part:
            return self.h_in, Tok()
        hcur = out_d if out_d is not None else p.dram("hcur", [128, KC, self.T])
        hcur_t = Tok()
        if out_d is not None:
            p.out_toks.append(hcur_t)
        g2, g2_t = p.sb([128, KC, 2]), Tok()
        p.dma("sp", g2[:], g2_d, writes=(g2_t,))
        for (b0, tb, sls) in self.blocks:
            for k in range(KC):
                acc, acc_t = self.accs[self.acc_i]
                self.acc_i = (self.acc_i + 1) % len(self.accs)
                p.dma("sp", acc[:, :tb], self.part_d[0, :, k, b0:b0 + tb], writes=(acc_t,))
                for j in range(1, self.n_part):
                    t2, t2_t = self.hchunk()
                    p.dma("sp", t2[:, :tb], self.part_d[j, :, k, b0:b0 + tb], writes=(t2_t,))
                    p.op("pool" if j % 2 else "dve", lambda e, acc=acc, t2=t2, tb=tb: e.tensor_tensor(
                        out=acc[:, :tb], in0=acc[:, :tb], in1=t2[:, :tb], op=ALU.add),
                        reads=(t2_t, acc_t), writes=(acc_t,))
                t2, t2_t = self.hchunk()
                p.dma("sp", t2[:, :tb], self.h_in[:, k, b0:b0 + tb], writes=(t2_t,))
                for (s0, n, kind) in sls:
                    p.op("dve", lambda e, acc=acc, t2=t2, s0=s0, n=n, k=k, kind=kind: e.scalar_tensor_tensor(
                        out=acc[:, s0:s0 + n], in0=acc[:, s0:s0 + n], scalar=g2[:, k, kind:kind + 1],
                        in1=t2[:, s0:s0 + n], op0=ALU.mult, op1=ALU.add),
                        reads=(acc_t, t2_t, g2_t), writes=(acc_t,))
                p.dma("sp", hcur[:, k, b0:b0 + tb], acc[:, :tb], reads=(acc_t,), writes=(hcur_t,), join=True)
        return hcur, hcur_t

    def rms(self, h_d, h_t, b0, tb):
        p = self.p
        ps_, ps_t = self.bank()
        ps2_, ps2_t = self.bank()
        for k in range(KC):
            hc, hc_t = self.hchunk()
            p.dma("sp", hc[:, :tb], h_d[:, k, b0:b0 + tb], reads=(h_t,), writes=(hc_t,))
            sq, sq_t = self.sq[self.sq_i]
            self.sq_i ^= 1
            p.op("act", lambda e, sq=sq, hc=hc, tb=tb: e.activation(out=sq[:, :tb], in_=hc[:, :tb], func=AF.Square),
                 reads=(hc_t,), writes=(sq_t,))
            p.op("pe", lambda e, sq=sq, k=k, ps_=ps_: e.matmul(
                ps_[:, 0:512], lhsT=self.ones_bf[:], rhs=sq[:, 0:512], start=(k == 0), stop=(k == KC - 1)),
                reads=(sq_t, self.ones_t), writes=(ps_t,))
            if tb > 512:
                p.op("pe", lambda e, sq=sq, k=k, ps2_=ps2_, tb=tb: e.matmul(
                    ps2_[:, 0:tb - 512], lhsT=self.ones_bf[:], rhs=sq[:, 512:tb], start=(k == 0), stop=(k == KC - 1)),
                    reads=(sq_t, self.ones_t), writes=(ps2_t,))
        for (pp, pt, c0, n) in ((ps_, ps_t, 0, 512), (ps2_, ps2_t, 512, tb - 512)):
            if n <= 0:
                continue
            p.op("act", lambda e, pp=pp, c0=c0, n=n: e.activation(
                out=self.rstd[:, c0:c0 + n], in_=pp[:, 0:n], func=AF.Sqrt, scale=1.0 / D, bias=self.eps_c[:, 0:1]),
                reads=(pt, self.eps_t), writes=(self.rstd_t,))
            p.op("dve", lambda e, c0=c0, n=n: e.reciprocal(out=self.rstd[:, c0:c0 + n], in_=self.rstd[:, c0:c0 + n]),
                 reads=(self.rstd_t,), writes=(self.rstd_t,))

    def norm_mod(self, h_d, h_t, b0, tb, sls, which, out, out_t):
        p = self.p
        self.rms(h_d, h_t, b0, tb)
        for k in range(KC):
            hc, hc_t = self.hchunk()
            p.dma("sp", hc[:, :tb], h_d[:, k, b0:b0 + tb], reads=(h_t,), writes=(hc_t,))
            p.op("dve", lambda e, hc=hc, tb=tb: e.tensor_tensor(
                out=hc[:, :tb], in0=hc[:, :tb], in1=self.rstd[:, :tb], op=ALU.mult),
                reads=(hc_t, self.rstd_t), writes=(hc_t,))
            for (s0, n, kind) in sls:
                p.op("act", lambda e, hc=hc, s0=s0, n=n, k=k, kind=kind: e.activation(
                    out=out[:, k, s0:s0 + n], in_=hc[:, s0:s0 + n], func=AF.Identity,
                    scale=self.gs[:, k, kind * 2 + which:kind * 2 + which + 1],
                    bias=self.modv[:, k, kind * 6 + which * 3:kind * 6 + which * 3 + 1]),
                    reads=(hc_t, self.gs_t, self.modv_t), writes=(out_t,))

    def gelu(self, ps_ap, ps_t, out_ap, out_t, n):
        p = self.p
        t1, t1_t = self.tmp()
        t2, t2_t = self.tmp()
        p.op("act", lambda e: e.activation(out=t1[:, :n], in_=ps_ap, func=AF.Square), reads=(ps_t,), writes=(t1_t,))
        p.op("dve", lambda e: e.tensor_scalar(out=t1[:, :n], in0=t1[:, :n], scalar1=0.044715, scalar2=1.0,
                                              op0=ALU.mult, op1=ALU.add), reads=(t1_t,), writes=(t1_t,))
        p.op("dve", lambda e: e.tensor_tensor(out=t1[:, :n], in0=t1[:, :n], in1=ps_ap, op=ALU.mult),
             reads=(t1_t, ps_t), writes=(t1_t,))
        p.op("act", lambda e: e.activation(out=t2[:, :n], in_=t1[:, :n], func=AF.Sigmoid, scale=GELU_C),
             reads=(t1_t,), writes=(t2_t,))
        p.op("dve", lambda e: e.tensor_tensor(out=out_ap, in0=t2[:, :n], in1=ps_ap, op=ALU.mult),
             reads=(t2_t, ps_t), writes=(out_t,))

    def load_w(self, w_d, c0, ncols, W, W_ts):
        wv = w_d.rearrange("(k p) n -> p k n", p=128)
        for qi in range(ncols // 512):
            self.p.dma("pool", W[:, :, qi * 512:(qi + 1) * 512], wv[:, :, c0 + qi * 512:c0 + (qi + 1) * 512],
                       writes=(W_ts[qi],))

    def proj_fm(self, W, W_ts, nf, x, x_t, sls, cb, kc=KC, f0=0, col0=0):
        p = self.p
        for f in range(nf):
            cf = col0 + f * 128
            w_t = W_ts[cf // 512]
            for (s0, n, kind) in sls:
                ps_, ps_t = self.bank()
                for k in range(kc):
                    p.op("pe", lambda e, cf=cf, k=k, s0=s0, n=n, ps_=ps_: e.matmul(
                        ps_[:, 0:n], lhsT=W[:, k, cf:cf + 128], rhs=x[:, k, s0:s0 + n],
                        start=(k == 0), stop=(k == kc - 1)),
                        reads=(w_t, x_t), writes=(ps_t,), sig=(k == kc - 1))
                cb(f0 + f, s0, n, kind, ps_[:, 0:n], ps_t)

    def proj_residual(self, W, W_ts, y, y_t, h_d, h_t, hout_d, hout_t, b0, tb, sls, gate_m, kc=KC):
        p = self.p
        state = {}

        def cb(f, s0, n, kind, ps_ap, ps_t):
            if s0 == 0:
                hc, hc_t = self.hchunk()
                p.dma("sp", hc[:, :tb], h_d[:, f, b0:b0 + tb], reads=(h_t,), writes=(hc_t,))
                state["hc"] = (hc, hc_t)
            hc, hc_t = state["hc"]
            p.op("dve", lambda e: e.scalar_tensor_tensor(
                out=hc[:, s0:s0 + n], in0=ps_ap, scalar=self.modv[:, f, kind * 6 + gate_m:kind * 6 + gate_m + 1],
                in1=hc[:, s0:s0 + n], op0=ALU.mult, op1=ALU.add),
                reads=(ps_t, hc_t, self.modv_t), writes=(hc_t,))
            if s0 + n == tb:
                p.dma("sp", hout_d[:, f, b0:b0 + tb], hc[:, :tb], reads=(hc_t,), writes=(hout_t,), join=True)
        self.proj_fm(W, W_ts, KC, y, y_t, sls, cb, kc=kc)

    def router_setup(self):
        p = self.p
        self.wr_d = p.dram_in("w_r", [128, KC, 36])
        self.br_d = p.dram_in("b_r", [128, 36])
        self.wr, self.wr_t = p.sb([128, KC, 36]), Tok()
        self.br, self.br_t = p.sb([128, 36]), Tok()
        p.dma("sp", self.wr[:], self.wr_d, writes=(self.wr_t,))
        p.dma("sp", self.br[:], self.br_d, writes=(self.br_t,))
        self.rt = [(p.sb([128, 128]), Tok()) for _ in range(2)]
        self.rt_i = 0

    def router(self, mxf, mxf_ts, c0, comb_d, row0):
        p = self.p
        ps_, ps_t = self.bank()
        for k in range(KC):
            p.op("pe", lambda e, k=k: e.matmul(ps_[:, 0:36], lhsT=mxf[:, k, c0:c0 + 128], rhs=self.wr[:, k, :],
                                               start=(k == 0), stop=(k == KC - 1)),
                 reads=tuple(mxf_ts[0:3]) + (self.wr_t,), writes=(ps_t,), sig=(k == KC - 1))
        R, R_t = self.rt[self.rt_i]
        self.rt_i ^= 1
        lg = R[:, 0:36]
        gmax, ngmax, gsum, grw = R[:, 36:37], R[:, 37:38], R[:, 38:39], R[:, 39:40]
        ge, oh, ohw = R[:, 40:44], R[:, 44:48], R[:, 48:52]
        sel, mask1, sel2, mask2 = R[:, 52:60], R[:, 60:68], R[:, 68:76], R[:, 76:84]
        m1, m2, dd, e2, w1, w2 = R[:, 84:85], R[:, 85:86], R[:, 86:87], R[:, 87:88], R[:, 88:89], R[:, 89:90]
        ing = R[:, 90:98]
        C, C_t = self.tmp()
        comb = C[:, 0:32]
        rw = (R_t,)

        def dv(fn, reads=rw, writes=rw, eng="dve"):
            p.op(eng, fn, reads=reads, writes=writes)
        dv(lambda e: e.tensor_tensor(out=lg, in0=ps_[:, 0:36], in1=self.br[:], op=ALU.add), reads=(ps_t, self.br_t))
        dv(lambda e: e.tensor_reduce(out=gmax, in_=lg[:, 0:4], axis=AX.X, op=ALU.max))
        dv(lambda e: e.tensor_scalar(out=ngmax, in0=gmax, scalar1=-1.0, scalar2=None, op0=ALU.mult))
        dv(lambda e: e.activation(out=ge, in_=lg[:, 0:4], func=AF.Exp, bias=ngmax, scale=1.0), eng="act")
        dv(lambda e: e.tensor_reduce(out=gsum, in_=ge, axis=AX.X, op=ALU.add))
        dv(lambda e: e.reciprocal(out=grw, in_=gsum))
        dv(lambda e: e.tensor_scalar(out=oh, in0=lg[:, 0:4], scalar1=gmax, scalar2=None, op0=ALU.is_equal))
        dv(lambda e: e.tensor_scalar(out=ohw, in0=oh, scalar1=grw, scalar2=None, op0=ALU.mult))
        dv(lambda e: e.tensor_scalar(out=sel, in0=lg[:, 4:12], scalar1=oh[:, 0:1], scalar2=None, op0=ALU.mult))
        for g in range(1, 4):
            dv(lambda e, g=g: e.scalar_tensor_tensor(out=sel, in0=lg[:, 4 + 8 * g:12 + 8 * g], scalar=oh[:, g:g + 1],
                                                     in1=sel, op0=ALU.mult, op1=ALU.add))
        dv(lambda e: e.tensor_reduce(out=m1, in_=sel, axis=AX.X, op=ALU.max))
        dv(lambda e: e.tensor_scalar(out=mask1, in0=sel, scalar1=m1, scalar2=None, op0=ALU.is_equal))
        dv(lambda e: e.scalar_tensor_tensor(out=sel2, in0=mask1, scalar=-1e30, in1=sel, op0=ALU.mult, op1=ALU.add))
        dv(lambda e: e.tensor_reduce(out=m2, in_=sel2, axis=AX.X, op=ALU.max))
        dv(lambda e: e.tensor_scalar(out=mask2, in0=sel2, scalar1=m2, scalar2=None, op0=ALU.is_equal))
        dv(lambda e: e.tensor_tensor(out=dd, in0=m2, in1=m1, op=ALU.subtract))
        dv(lambda e: e.activation(out=e2, in_=dd, func=AF.Exp), eng="act")
        dv(lambda e: e.tensor_scalar(out=w1, in0=e2, scalar1=1.0, scalar2=None, op0=ALU.add))
        dv(lambda e: e.reciprocal(out=w1, in_=w1))
        dv(lambda e: e.tensor_tensor(out=w2, in0=e2, in1=w1, op=ALU.mult))
        dv(lambda e: e.tensor_scalar(out=ing, in0=mask1, scalar1=w1, scalar2=None, op0=ALU.mult))
        dv(lambda e: e.scalar_tensor_tensor(out=ing, in0=mask2, scalar=w2, in1=ing, op0=ALU.mult, op1=ALU.add))
        for g in range(4):
            p.op("dve", lambda e, g=g: e.tensor_scalar(out=comb[:, 8 * g:8 * g + 8], in0=ing, scalar1=ohw[:, g:g + 1],
                                                       scalar2=None, op0=ALU.mult), reads=rw, writes=(C_t,) if g == 0 else (C_t,))
        p.out_dma("sp", comb_d[row0:row0 + 128, :], comb, reads=(C_t,))

    def norm2_router(self, h_d, h_t, b0, tb, sls, mxf, mxf_ts, mxb, mxb_t, mx_d, comb_d):
        p = self.p
        self.rms(h_d, h_t, b0, tb)
        for k in range(KC):
            hc, hc_t = self.hchunk()
            p.dma("sp", hc[:, :tb], h_d[:, k, b0:b0 + tb], reads=(h_t,), writes=(hc_t,))
            p.op("dve", lambda e, hc=hc: e.tensor_tensor(out=hc[:, :tb], in0=hc[:, :tb], in1=self.rstd[:, :tb], op=ALU.mult),
                 reads=(hc_t, self.rstd_t), writes=(hc_t,))
            for (s0, n, kind) in sls:
                p.op("act", lambda e, hc=hc, s0=s0, n=n, k=k, kind=kind: e.activation(
                    out=mxf[:, k, s0:s0 + n], in_=hc[:, s0:s0 + n], func=AF.Identity,
                    scale=self.gs[:, k, kind * 2 + 1:kind * 2 + 2], bias=self.modv[:, k, kind * 6 + 3:kind * 6 + 4]),
                    reads=(hc_t, self.gs_t, self.modv_t), writes=tuple(mxf_ts[0:3]))
            p.op("pool", lambda e, k=k: e.tensor_copy(out=mxb[:, k, :tb], in_=mxf[:, k, :tb]),
                 reads=tuple(mxf_ts[0:3]), writes=(mxb_t,))
        p.out_dma("sp", mx_d[:, :, b0:b0 + tb], mxb[:, :, :tb], reads=(mxb_t,))
        for jt in range(tb // 128):
            self.router(mxf, mxf_ts, jt * 128, comb_d, b0 + jt * 128)


def build_gmlp(T, n_part, stage=99):
    p = Prog()
    tp = TP(p, T, n_part)
    w_in = p.dram_in("w_in", [D, 2 * D])
    w_out = p.dram_in("w_out", [D, D])
    vgain_d = p.dram_in("vgain", [128, KC])
    wsT_d = p.dram_in("wsT", [128, 16, 128])
    bsb_d = p.dram_in("bsb", [128, 16, 128])
    g2_d = p.dram_in("g2prev", [128, KC, 2]) if n_part else None
    h1_d = p.dram_out("h1", [128, KC, T])
    mx_d = p.dram_out("mx", [128, KC, T], BF16)
    comb_d = p.dram_out("comb", [T, 32])
    h1_t = Tok()
    tp.router_setup()
    TB = tp.TB
    vgain, vgain_t = p.sb([128, KC]), Tok()
    wsT, wsT_t = p.sb([128, 16, 128], BF16), Tok()
    bsb, bsb_t = p.sb([128, 16, 128]), Tok()
    p.dma("sp", vgain[:], vgain_d, writes=(vgain_t,))
    p.dma("pool", wsT[:], wsT_d, writes=(wsT_t,))
    p.dma("sp", bsb[:], bsb_d, writes=(bsb_t,))
    W = p.sb([128, KC, 2048], BF16)
    W_ts = [Tok() for _ in range(4)]
    mxf = W[:].bitcast(F32)
    ax, ax_t = p.sb([128, KC, TB], BF16), Tok()
    uT, uT_t = p.sb([128, KC, TB], BF16), Tok()
    vt, vt_t = p.sb([128, 2048]), Tok()
    vsq, vsq_t = p.sb([128, 2048]), Tok()
    vh, vh_t = p.sb([128, 2048], BF16), Tok()
    sm, sm_t = p.sb([128, 4]), Tok()
    hcur, hcur_t = tp.prologue(g2_d)
    for (b0, tb, sls) in tp.blocks:
        tp.norm_mod(hcur, hcur_t, b0, tb, sls, 0, ax, ax_t)
        if stage == 1:
            p.out_dma("sp", mx_d[:, :, b0:b0 + tb], ax[:, :, :tb], reads=(ax_t,))
            continue
        tp.load_w(w_in, 0, 2048, W, W_ts)
        tp.proj_fm(W, W_ts, KC, ax, ax_t, sls,
                   lambda f, s0, n, kind, ps_ap, ps_t: tp.gelu(ps_ap, ps_t, uT[:, f, s0:s0 + n], uT_t, n))
        if stage == 2:
            p.out_dma("sp", mx_d[:, :, b0:b0 + tb], uT[:, :, :tb], reads=(uT_t,))
            continue
        tp.load_w(w_in, 2048, 2048, W, W_ts)
        for jt in range(tb // 128):
            c0 = jt * 128
            for nsl in range(4):
                ps_, ps_t = tp.bank()
                for k in range(KC):
                    p.op("pe", lambda e, k=k, nsl=nsl, c0=c0, ps_=ps_: e.matmul(
                        ps_[:, :], lhsT=ax[:, k, c0:c0 + 128], rhs=W[:, k, nsl * 512:(nsl + 1) * 512],
                        start=(k == 0), stop=(k == KC - 1)),
                        reads=(ax_t, W_ts[nsl]), writes=(ps_t,), sig=(k == KC - 1))
                tp.gelu(ps_[:, :], ps_t, vt[:, nsl * 512:(nsl + 1) * 512], vt_t, 512)
            p.op("act", lambda e: e.activation(out=vsq[:], in_=vt[:], func=AF.Square), reads=(vt_t,), writes=(vsq_t,))
            p.op("dve", lambda e: e.tensor_reduce(out=sm[:, 0:1], in_=vsq[:], axis=AX.X, op=ALU.add),
                 reads=(vsq_t,), writes=(sm_t,))
            p.op("act", lambda e: e.activation(out=sm[:, 1:2], in_=sm[:, 0:1], func=AF.Sqrt, scale=1.0 / D,
                                               bias=tp.eps_c[:, 0:1]), reads=(sm_t, tp.eps_t), writes=(sm_t,))
            p.op("dve", lambda e: e.reciprocal(out=sm[:, 2:3], in_=sm[:, 1:2]), reads=(sm_t,), writes=(sm_t,))
            p.op("dve", lambda e: e.tensor_scalar(out=vh[:], in0=vt[:], scalar1=sm[:, 2:3], scalar2=None, op0=ALU.mult),
                 reads=(sm_t, vt_t), writes=(vh_t,))
            for g4 in range(4):
                ps_, ps_t = tp.bank()
                for gi in range(4):
                    g = g4 * 4 + gi
                    p.op("pe", lambda e, g=g, gi=gi, ps_=ps_: e.matmul(
                        ps_[:, gi * 128:(gi + 1) * 128], lhsT=vh[:, g * 128:(g + 1) * 128], rhs=wsT[:, g, :],
                        start=True, stop=True), reads=(vh_t, wsT_t), writes=(ps_t,), sig=(gi == 3))
                t1, t1_t = tp.tmp()
                for gi in range(4):
                    g = g4 * 4 + gi
                    p.op("dve", lambda e, g=g, gi=gi, ps_=ps_, t1=t1: e.scalar_tensor_tensor(
                        out=t1[:, gi * 128:(gi + 1) * 128], in0=ps_[:, gi * 128:(gi + 1) * 128], scalar=vgain[:, g:g + 1],
                        in1=bsb[:, g, :], op0=ALU.mult, op1=ALU.add), reads=(ps_t, vgain_t, bsb_t), writes=(t1_t,))
                    p.op("dve", lambda e, g=g, gi=gi, t1=t1, c0=c0: e.tensor_tensor(
                        out=uT[:, g, c0:c0 + 128], in0=t1[:, gi * 128:(gi + 1) * 128], in1=uT[:, g, c0:c0 + 128], op=ALU.mult),
                        reads=(t1_t, uT_t), writes=(uT_t,))
        if stage == 3:
            p.out_dma("sp", mx_d[:, :, b0:b0 + tb], uT[:, :, :tb], reads=(uT_t,))
            continue
        tp.load_w(w_out, 0, 2048, W, W_ts)
        tp.proj_residual(W, W_ts, uT, uT_t, hcur, hcur_t, h1_d, h1_t, b0, tb, sls, 2)
        if stage == 4:
            continue
        tp.norm2_router(h1_d, h1_t, b0, tb, sls, mxf, W_ts, ax, ax_t, mx_d, comb_d)
    for t in (h1_t,):
        p.out_toks.append(t)
    return p


def fm(x):
    T = x.shape[0]
    return np.ascontiguousarray(x.T.reshape(KC, 128, T).transpose(1, 0, 2))


def unfm(a):
    T = a.shape[2]
    return np.ascontiguousarray(a.transpose(1, 0, 2).reshape(KC * 128, T).T)


def vfm(v):
    v = np.asarray(v)
    lead = v.shape[:-1]
    r = v.reshape(lead + (KC, 128))
    r = np.moveaxis(r, (-1, -2), (0, 1))
    return np.ascontiguousarray(r)


FE = 768
FC = 6


def build_moe(TA, n_exp=4):
    p = Prog()
    nb = TA // 512
    mx_d = p.dram_in("mx", [128, KC, TA], BF16)
    comb_d = p.dram_in("combT", [n_exp, TA])
    wg_d = p.dram_in("wg", [n_exp, D, FE])
    wu_d = p.dram_in("wu", [n_exp, D, FE])
    wd_d = p.dram_in("wd", [n_exp, FE, D])
    part_d = p.dram_out("part", [128, KC, TA])
    h1s = p.dram("h1s", [n_exp, 128, FC, TA], BF16)
    h1s_t = Tok()
    WB = p.sb([128, 4 * FC, 2048], BF16)
    XB = p.sb([128, 48, 512], BF16)
    pss = [(p.ps(), Tok()) for _ in range(8)]
    tmps = [(p.sb([128, 512]), Tok()) for _ in range(6)]
    cbs = [(p.sb([128, 512]), Tok()) for _ in range(2)]
    h1b = [(p.sb([128, FC, 512], BF16), Tok()) for _ in range(2)]
    st = {"ps": 0, "tmp": 0}

    def bank():
        r = pss[st["ps"]]
        st["ps"] = (st["ps"] + 1) % 8
        return r

    def tmp():
        r = tmps[st["tmp"]]
        st["tmp"] = (st["tmp"] + 1) % 6
        return r

    def wview(s, which):
        a = WB[:, s * 12 + which * 6:s * 12 + which * 6 + 6, :]
        return a.rearrange("p a b -> p (a b)").rearrange("p (k n) -> p k n", n=FE)

    w_ts = [[[Tok() for _ in range(4)] for _ in range(2)] for _ in range(2)]
    x_ts = [Tok() for _ in range(3)]
    wd_ts = [Tok() for _ in range(n_exp * FC)]
    wd_early = set()

    def load_wd(e):
        wv = wd_d[e].rearrange("(c p) n -> p c n", p=128)
        alias = tuple(w_ts[e // 2][e % 2]) if n_exp == 4 else tuple(t for a in w_ts for b_ in a for t in b_)
        for c2 in range(0, FC, 2):
            j = e * FC + c2
            p.dma("pool", WB[:, j:j + 2, :], wv[:, c2:c2 + 2, :], writes=(wd_ts[j], wd_ts[j + 1]) + alias)

    for e in range(n_exp):
        s = e % 2
        if n_exp == 4 and e == 3:
            pass
        for which, wsrc in ((0, wg_d), (1, wu_d)):
            wv = wsrc[e].rearrange("(k p) n -> p k n", p=128)
            dst = wview(s, which)
            for kk in range(0, KC, 4):
                p.dma("pool", dst[:, kk:kk + 4, :], wv[:, kk:kk + 4, :], writes=(w_ts[s][which][kk // 4],))
        Wg, Wu = wview(s, 0), wview(s, 1)
        if n_exp == 4 and e == 3:
            for e2 in (0, 1):
                load_wd(e2)
                wd_early.add(e2)
        for b in range(nb):
            b0 = b * 512
            xi = (e * nb + b) % 3
            X, X_t = XB[:, 16 * xi:16 * xi + 16, :], x_ts[xi]
            p.dma("sp", X, mx_d[:, :, b0:b0 + 512], writes=(X_t,))
            cb, cb_t = cbs[(e * nb + b) % 2]
            p.dma("sp", cb[:], comb_d[e, b0:b0 + 512].partition_broadcast(128), writes=(cb_t,))
            hb, hb_t = h1b[(e * nb + b) % 2]
            for f in range(FC):
                psg, psg_t = bank()
                psu, psu_t = bank()
                for k in range(KC):
                    p.op("pe", lambda en, k=k, f=f, psg=psg, Wg=Wg, X=X: en.matmul(
                        psg[:, :], lhsT=Wg[:, k, f * 128:(f + 1) * 128], rhs=X[:, k, :], start=(k == 0), stop=(k == KC - 1)),
                        reads=(w_ts[s][0][k // 4], X_t), writes=(psg_t,), sig=(k == KC - 1))
                for k in range(KC):
                    p.op("pe", lambda en, k=k, f=f, psu=psu, Wu=Wu, X=X: en.matmul(
                        psu[:, :], lhsT=Wu[:, k, f * 128:(f + 1) * 128], rhs=X[:, k, :], start=(k == 0), stop=(k == KC - 1)),
                        reads=(w_ts[s][1][k // 4], X_t), writes=(psu_t,), sig=(k == KC - 1))
                t1, t1_t = tmp()
                t2, t2_t = tmp()
                p.op("act", lambda en, t1=t1, psg=psg: en.activation(out=t1[:], in_=psg[:, :], func=AF.Silu),
                     reads=(psg_t,), writes=(t1_t,))
                p.op("dve", lambda en, t2=t2, psu=psu, cb=cb: en.tensor_tensor(out=t2[:], in0=psu[:, :], in1=cb[:], op=ALU.mult),
                     reads=(psu_t, cb_t), writes=(t2_t,))
                p.op("dve" if f % 2 else "pool", lambda en, t1=t1, t2=t2, hb=hb, f=f: en.tensor_tensor(
                    out=hb[:, f, :], in0=t1[:], in1=t2[:], op=ALU.mult), reads=(t1_t, t2_t), writes=(hb_t,))
            p.dma("sp", h1s[e, :, :, b0:b0 + 512], hb[:], reads=(hb_t,), writes=(h1s_t,), join=True)
    for e in range(n_exp):
        if e not in wd_early:
            load_wd(e)
    h_ts = [Tok() for _ in range(2)]
    NJ = n_exp * FC
    for b in range(nb):
        b0 = b * 512
        H, H_t = XB[:, 24 * (b % 2):24 * (b % 2) + NJ, :], h_ts[b % 2]
        for e in range(n_exp):
            p.dma("sp", H[:, e * FC:(e + 1) * FC, :], h1s[e, :, :, b0:b0 + 512], reads=(h1s_t,),
                  writes=(H_t,) + (tuple(x_ts) if b < 2 else ()), join=(e > 0))
        for d in range(KC):
            ps_, ps_t = bank()
            for j in range(NJ):
                p.op("pe", lambda en, j=j, d=d, ps_=ps_, H=H: en.matmul(
                    ps_[:, :], lhsT=WB[:, j, d * 128:(d + 1) * 128], rhs=H[:, j, :], start=(j == 0), stop=(j == NJ - 1)),
                    reads=(wd_ts[j], H_t), writes=(ps_t,), sig=(j == NJ - 1))
            t1, t1_t = tmp()
            p.op("act" if d % 2 else "dve", (lambda en, t1=t1, ps_=ps_: en.activation(out=t1[:], in_=ps_[:, :], func=AF.Identity))
                 if d % 2 else (lambda en, t1=t1, ps_=ps_: en.tensor_copy(out=t1[:], in_=ps_[:, :])),
                 reads=(ps_t,), writes=(t1_t,))
            p.out_dma("sp", part_d[:, d, b0:b0 + 512], t1[:], reads=(t1_t,))
    return p


ATT = {
    "gqa": dict(M=1, dh=128, nq=16, nk=8, nv=1024, wcols=4096),
    "diff": dict(M=2, dh=64, nq=16, nk=16, nv=2048, wcols=6144),
}


def build_attn_a(kind, n_part):
    cfg = ATT[kind]
    T = 1152
    p = Prog()
    tp = TP(p, T, n_part, n_hch=6)
    nq, nk, nv, dh = cfg["nq"], cfg["nk"], cfg["nv"], cfg["dh"]
    w_d = p.dram_in("w_qkv", [D, cfg["wcols"]])
    gains_d = p.dram_in("qk_gain", [128, 2])
    onesn_d = p.dram_in("onesn", [128, 128])
    rot_d = p.dram_in("rotm", [128, 128])
    cos_d = p.dram_in("cosT", [128, 1024])
    sin_d = p.dram_in("sinT", [128, 1024])
    g2_d = p.dram_in("g2prev", [128, KC, 2]) if n_part else None
    qT_d = p.dram_out("qT", [128, nq, T], BF16)
    kT_d = p.dram_out("kT", [128, nk, T], BF16)
    v_d = p.dram_out("v", [T, nv], BF16)
    h0_d = p.dram_out("h0", [128, KC, T]) if n_part else None
    TB = tp.TB
    gains, gains_t = p.sb([128, 2]), Tok()
    onesn, onesn_t = p.sb([128, 128], BF16), Tok()
    rot, rot_t = p.sb([128, 128]), Tok()
    cosT, cos_t = p.sb([128, 1024]), Tok()
    sinT, sin_t = p.sb([128, 1024]), Tok()
    p.dma("sp", gains[:], gains_d, writes=(gains_t,))
    p.dma("pool", onesn[:], onesn_d, writes=(onesn_t,))
    p.dma("sp", rot[:], rot_d, writes=(rot_t,))
    p.dma("sp", cosT[:], cos_d, writes=(cos_t,))
    p.dma("sp", sinT[:], sin_d, writes=(sin_t,))
    W = p.sb([128, KC, 2048], BF16)
    W_ts = [Tok() for _ in range(4)]
    ax, ax_t = p.sb([128, KC, TB], BF16), Tok()
    ob = [(p.sb([128, 512], BF16), Tok()) for _ in range(3)]
    vb = [(p.sb([128, 512], BF16), Tok()) for _ in range(3)]
    qn = [(p.sb([128, 512]), Tok()) for _ in range(2)]
    st = {"o": 0, "v": 0, "q": 0}
    hcur, hcur_t = tp.prologue(g2_d, h0_d)

    def qk_cb(which, out_d, fbase):
        def cb(f, s0, n, kd, ps_ap, ps_t):
            b0 = cb.b0
            sq, sq_t = tp.tmp()
            sqb = sq[:].bitcast(BF16)[:, 0:n]
            p.op("act", lambda e: e.activation(out=sqb, in_=ps_ap, func=AF.Square), reads=(ps_t,), writes=(sq_t,))
            ps2, ps2_t = tp.bank()
            p.op("pe", lambda e: e.matmul(ps2[:, 0:n], lhsT=onesn[:], rhs=sqb, start=True, stop=True),
                 reads=(sq_t, onesn_t), writes=(ps2_t,))
            r, r_t = tp.tmp()
            p.op("act", lambda e: e.activation(out=r[:, :n], in_=ps2[:, 0:n], func=AF.Sqrt, scale=1.0 / dh,
                                               bias=tp.eps_c[:, 0:1]), reads=(ps2_t, tp.eps_t), writes=(r_t,))
            p.op("dve", lambda e: e.reciprocal(out=r[:, :n], in_=r[:, :n]), reads=(r_t,), writes=(r_t,))
            q_, q_t = qn[st["q"]]
            st["q"] ^= 1
            p.op("dve", lambda e: e.scalar_tensor_tensor(out=q_[:, :n], in0=ps_ap, scalar=gains[:, which:which + 1],
                                                         in1=r[:, :n], op0=ALU.mult, op1=ALU.mult),
                 reads=(ps_t, r_t, gains_t), writes=(q_t,))
            o_, o_t = ob[st["o"]]
            st["o"] = (st["o"] + 1) % 3
            if kd == 0:
                c0 = b0 + s0
                ps3, ps3_t = tp.bank()
                p.op("pe", lambda e: e.matmul(ps3[:, 0:n], lhsT=rot[:], rhs=q_[:, :n], start=True, stop=True),
                     reads=(q_t, rot_t), writes=(ps3_t,))
                t2, t2_t = tp.tmp()
                p.op("dve", lambda e: e.tensor_tensor(out=t2[:, :n], in0=ps3[:, 0:n], in1=sinT[:, c0:c0 + n], op=ALU.mult),
                     reads=(ps3_t, sin_t), writes=(t2_t,))
                p.op("pool", lambda e: e.tensor_tensor(out=q_[:, :n], in0=q_[:, :n], in1=cosT[:, c0:c0 + n], op=ALU.mult),
                     reads=(q_t, cos_t), writes=(q_t,))
                p.op("dve", lambda e: e.tensor_tensor(out=o_[:, :n], in0=q_[:, :n], in1=t2[:, :n], op=ALU.add),
                     reads=(q_t, t2_t), writes=(o_t,))
            else:
                p.op("dve", lambda e: e.tensor_copy(out=o_[:, :n], in_=q_[:, :n]), reads=(q_t,), writes=(o_t,))
            p.out_dma("sp", out_d[:, f - fbase, b0 + s0:b0 + s0 + n], o_[:, :n], reads=(o_t,))
        return cb

    for (b0, tb, sls) in tp.blocks:
        tp.norm_mod(hcur, hcur_t, b0, tb, sls, 0, ax, ax_t)
        for wbase in range(0, cfg["wcols"], 2048):
            tp.load_w(w_d, wbase, 2048, W, W_ts)
            cbase = wbase // 128
            for (which, lo, hi, out_d) in ((0, 0, nq, qT_d), (1, nq, nq + nk, kT_d)):
                a, b_ = max(lo, cbase), min(hi, cbase + 16)
                if a < b_:
                    cb = qk_cb(which, out_d, lo)
                    cb.b0 = b0
                    tp.proj_fm(W, W_ts, b_ - a, ax, ax_t, sls, cb, f0=a, col0=(a - cbase) * 128)
            va, vb_ = max((nq + nk) * 128, wbase), min(cfg["wcols"], wbase + 2048)
            if va < vb_:
                for jt in range(tb // 128):
                    c0 = jt * 128
                    for cc in range(va, vb_, 512):
                        ps_, ps_t = tp.bank()
                        for k in range(KC):
                            p.op("pe", lambda e, k=k, c0=c0, cc=cc, ps_=ps_: e.matmul(
                                ps_[:, :], lhsT=ax[:, k, c0:c0 + 128], rhs=W[:, k, cc - wbase:cc - wbase + 512],
                                start=(k == 0), stop=(k == KC - 1)),
                                reads=(ax_t, W_ts[(cc - wbase) // 512]), writes=(ps_t,), sig=(k == KC - 1))
                        v_, v_t = vb[st["v"]]
                        st["v"] = (st["v"] + 1) % 3
                        p.op("act", lambda e, v_=v_, ps_=ps_: e.activation(out=v_[:], in_=ps_[:, :], func=AF.Identity),
                             reads=(ps_t,), writes=(v_t,))
                        vc = cc - (nq + nk) * 128
                        p.out_dma("sp", v_d[b0 + c0:b0 + c0 + 128, vc:vc + 512], v_[:], reads=(v_t,))
    return p


def build_attn_b(kind, T):
    cfg = ATT[kind]
    M, dh, nq, nk, nv = cfg["M"], cfg["dh"], cfg["nq"], cfg["nk"], cfg["nv"]
    TK = 2304
    NT = TK // 128
    CTX_TILES = (8, 17)
    p = Prog()
    tp = TP(p, T, 0)
    qT_d = p.dram_in("qT", [128, nq, T], BF16)
    kT_d = p.dram_in("kT", [128, nk, TK], BF16)
    v_d = p.dram_in("v", [TK, nv], BF16)
    wo_d = p.dram_in("w_o", [D, D])
    h1_d = p.dram_out("h1", [128, KC, T])
    mx_d = p.dram_out("mx", [128, KC, T], BF16)
    comb_d = p.dram_out("comb", [T, 32])
    h1_t = Tok()
    tp.router_setup()
    TB = tp.TB
    if kind == "diff":
        lam_d = p.dram_in("lamv", [128, 4, 64])
        subg_d = p.dram_in("subg", [128, 2])
        lamv, lamv_t = p.sb([128, 4, 64]), Tok()
        subg, subg_t = p.sb([128, 2]), Tok()
        lam, lam_t = p.sb([128, 8]), Tok()
        p.dma("sp", lamv[:], lam_d, writes=(lamv_t,))
        p.dma("sp", subg[:], subg_d, writes=(subg_t,))
        lt = (lam_t,)
        p.op("dve", lambda e: e.tensor_tensor(out=lamv[:, 0, :], in0=lamv[:, 0, :], in1=lamv[:, 1, :], op=ALU.mult),
             reads=(lamv_t,), writes=(lamv_t,))
        p.op("dve", lambda e: e.tensor_tensor(out=lamv[:, 2, :], in0=lamv[:, 2, :], in1=lamv[:, 3, :], op=ALU.mult),
             reads=(lamv_t,), writes=(lamv_t,))
        p.op("dve", lambda e: e.tensor_reduce(out=lam[:, 0:1], in_=lamv[:, 0, :], axis=AX.X, op=ALU.add), reads=(lamv_t,), writes=lt)
        p.op("dve", lambda e: e.tensor_reduce(out=lam[:, 1:2], in_=lamv[:, 2, :], axis=AX.X, op=ALU.add), reads=(lamv_t,) + lt, writes=lt)
        p.op("act", lambda e: e.activation(out=lam[:, 2:4], in_=lam[:, 0:2], func=AF.Exp), reads=lt, writes=lt)
        p.op("dve", lambda e: e.tensor_tensor(out=lam[:, 4:5], in0=lam[:, 3:4], in1=lam[:, 2:3], op=ALU.subtract), reads=lt, writes=lt)
        p.op("dve", lambda e: e.tensor_tensor(out=lam[:, 5:6], in0=lam[:, 4:5], in1=subg[:, 1:2], op=ALU.subtract),
             reads=lt + (subg_t,), writes=lt)
        p.op("dve", lambda e: e.tensor_scalar(out=lam[:, 6:7], in0=subg[:, 1:2], scalar1=-1.0, scalar2=1.0, op0=ALU.mult, op1=ALU.add),
             reads=lt + (subg_t,), writes=lt)
        p.op("dve", lambda e: e.tensor_tensor(out=lam[:, 6:7], in0=lam[:, 6:7], in1=subg[:, 0:1], op=ALU.mult),
             reads=lt + (subg_t,), writes=lt)
    W = p.sb([128, KC, 2048], BF16)
    W_ts = [Tok() for _ in range(4)]
    mxf = W[:].bitcast(F32)
    oT, oT_t = p.sb([128, KC, TB], BF16), Tok()
    mxb, mxb_t = p.sb([128, KC, TB], BF16), Tok()
    kb = [(p.sb([128, TK], BF16), Tok()) for _ in range(2)]
    vbuf = [(p.sb([128, NT, 128], BF16), Tok()) for _ in range(2)]
    qb = [(p.sb([128, TB], BF16), Tok()) for _ in range(2)]
    pb = [(p.sb([128, 512], BF16), Tok()) for _ in range(4)]
    accs = tp.pss[0:4]
    sbanks = tp.pss[4:8]
    st = {"s": 0, "p": 0}

    def sbank():
        r = sbanks[st["s"]]
        st["s"] = (st["s"] + 1) % 4
        return r
    tp.pss = sbanks
    tp.ps_i = 0
    scale = 1.0 / float(np.sqrt(dh))
    for bi, (b0, tb, sls) in enumerate(tp.blocks):
        for h in range(nq):
            hi = (bi * nq + h) % 2
            kh = h // (nq // nk)
            vh = h if nv == 2048 else h // 2
            K_, K_t = kb[hi]
            V_, V_t = vbuf[hi]
            Q_, Q_t = qb[hi]
            p.dma("sp", K_[:], kT_d[:, kh, :], writes=(K_t,))
            p.dma("sp", V_[:], v_d[:, vh * 128:(vh + 1) * 128].rearrange("(j t) d -> t j d", t=128), writes=(V_t,))
            p.dma("sp", Q_[:, :tb], qT_d[:, h, b0:b0 + tb], writes=(Q_t,))
            for (s0, n, kd) in sls:
                tiles = list(range(NT)) if kd == 0 else list(CTX_TILES)
                for ji, j in enumerate(tiles):
                    first, last = ji == 0, ji == len(tiles) - 1
                    for m in range(M):
                        pm = slice(m * 64, m * 64 + 64) if M == 2 else slice(0, 128)
                        S_, S_t = sbank()
                        p.op("pe", lambda e, S_=S_, pm=pm, j=j, K_=K_, Q_=Q_, s0=s0, n=n: e.matmul(
                            S_[:, 0:n], lhsT=K_[pm, j * 128:(j + 1) * 128], rhs=Q_[pm, s0:s0 + n], start=True, stop=True),
                            reads=(K_t, Q_t), writes=(S_t,))
                        P_, P_t = pb[st["p"]]
                        st["p"] = (st["p"] + 1) % 4
                        p.op("act", lambda e, S_=S_, P_=P_, n=n: e.activation(out=P_[:, :n], in_=S_[:, 0:n], func=AF.Exp, scale=scale),
                             reads=(S_t,), writes=(P_t,))
                        O_, O_t = accs[2 * m]
                        Dn, Dn_t = accs[2 * m + 1]
                        p.op("pe", lambda e, O_=O_, V_=V_, P_=P_, j=j, n=n, first=first, last=last: e.matmul(
                            O_[:, 0:n], lhsT=V_[:, j, :], rhs=P_[:, :n], start=first, stop=last),
                            reads=(V_t, P_t), writes=(O_t,))
                        p.op("pe", lambda e, Dn=Dn, P_=P_, n=n, first=first, last=last: e.matmul(
                            Dn[:, 0:n], lhsT=tp.ones_bf[:], rhs=P_[:, :n], start=first, stop=last),
                            reads=(tp.ones_t, P_t), writes=(Dn_t,))
                outs = []
                for m in range(M):
                    O_, O_t = accs[2 * m]
                    Dn, Dn_t = accs[2 * m + 1]
                    r, r_t = tp.tmp()
                    p.op("dve", lambda e, r=r, Dn=Dn, n=n: e.reciprocal(out=r[:, :n], in_=Dn[:, 0:n]), reads=(Dn_t,), writes=(r_t,))
                    if M == 1:
                        p.op("dve", lambda e, r=r, O_=O_, n=n, h=h, s0=s0: e.tensor_tensor(
                            out=oT[:, h, s0:s0 + n], in0=O_[:, 0:n], in1=r[:, :n], op=ALU.mult),
                            reads=(O_t, r_t), writes=(oT_t,))
                    else:
                        p.op("dve", lambda e, r=r, O_=O_, n=n: e.tensor_tensor(out=r[:, :n], in0=O_[:, 0:n], in1=r[:, :n], op=ALU.mult),
                             reads=(O_t, r_t), writes=(r_t,))
                        outs.append((r, r_t))
                if M == 2:
                    (o0, o0_t), (o1, o1_t) = outs
                    p.op("dve", lambda e, o0=o0, o1=o1, n=n: e.scalar_tensor_tensor(
                        out=o0[:, :n], in0=o1[:, :n], scalar=lam[:, 5:6], in1=o0[:, :n], op0=ALU.mult, op1=ALU.add),
                        reads=(o0_t, o1_t, lam_t), writes=(o0_t,))
                    sq, sq_t = tp.tmp()
                    sqb = sq[:].bitcast(BF16)[:, 0:n]
                    p.op("act", lambda e, sqb=sqb, o0=o0, n=n: e.activation(out=sqb, in_=o0[:, :n], func=AF.Square),
                         reads=(o0_t,), writes=(sq_t,))
                    ps2, ps2_t = sbank()
                    p.op("pe", lambda e, ps2=ps2, sqb=sqb, n=n: e.matmul(ps2[:, 0:n], lhsT=tp.ones_bf[:], rhs=sqb, start=True, stop=True),
                         reads=(sq_t, tp.ones_t), writes=(ps2_t,))
                    r2, r2_t = tp.tmp()
                    p.op("act", lambda e, r2=r2, ps2=ps2, n=n: e.activation(out=r2[:, :n], in_=ps2[:, 0:n], func=AF.Sqrt, scale=1.0 / 128,
                                                                          bias=tp.eps_c[:, 0:1]), reads=(ps2_t, tp.eps_t), writes=(r2_t,))
                    p.op("dve", lambda e, r2=r2, n=n: e.reciprocal(out=r2[:, :n], in_=r2[:, :n]), reads=(r2_t,), writes=(r2_t,))
                    p.op("dve", lambda e, o0=o0, r2=r2, n=n, h=h, s0=s0: e.scalar_tensor_tensor(
                        out=oT[:, h, s0:s0 + n], in0=o0[:, :n], scalar=lam[:, 6:7], in1=r2[:, :n], op0=ALU.mult, op1=ALU.mult),
                        reads=(o0_t, r2_t, lam_t), writes=(oT_t,))
        tp.load_w(wo_d, 0, 2048, W, W_ts)
        tp.proj_residual(W, W_ts, oT, oT_t, tp.h_in, Tok(), h1_d, h1_t, b0, tb, sls, 2)
        tp.norm2_router(h1_d, h1_t, b0, tb, sls, mxf, W_ts, mxb, mxb_t, mx_d, comb_d)
    p.out_toks.append(h1_t)
    return p


def build_final(T=1024, n_part=8):
    p = Prog()
    tp = TP(p, T, n_part, n_hch=8)
    g2_d = p.dram_in("g2prev", [128, KC, 2])
    out_d = p.dram_out("hout", [128, KC, T])
    tp.prologue(g2_d, out_d)
    return p


def rope_tables(dh, half_idx):
    t = np.arange(half_idx * 1024, half_idx * 1024 + 1024)
    row = (t // 64).astype(np.float32)
    col = (t % 64).astype(np.float32)
    hf = dh // 2
    inv = (10000.0 ** (-np.arange(0, hf, 2, dtype=np.float32) / hf)).astype(np.float32)
    ar = row[:, None] * inv[None, :]
    ac = col[:, None] * inv[None, :]
    ang = np.concatenate([ar, ar, ac, ac], -1)
    cos, sin = np.cos(ang).astype(np.float32), np.sin(ang).astype(np.float32)
    rep = 128 // dh
    cos, sin = np.tile(cos, (1, rep)), np.tile(sin, (1, rep))
    q = dh // 4
    rm = np.zeros((128, 128), np.float32)
    for base in range(0, 128, dh):
        for m in range(dh):
            blk = m // q
            if blk in (0, 2):
                rm[base + m + q, base + m] = -1.0
            else:
                rm[base + m - q, base + m] = 1.0
    ones = np.zeros((128, 128), np.float32)
    for base in range(0, 128, dh):
        ones[base:base + dh, base:base + dh] = 1.0
    return np.ascontiguousarray(cos.T), np.ascontiguousarray(sin.T), rm, ones


def _c(a):
    return np.ascontiguousarray(a)


def kernel(x, c, ctx, c_ctx, ada_w, ada_b, norm1_g, norm2_g,
           gmlp_w_in, gmlp_v_gain, gmlp_w_s, gmlp_b_s, gmlp_w_out,
           gqa_w_qkv, gqa_q_gain, gqa_k_gain, gqa_w_o,
           diff_w_qkv, diff_q_gain, diff_k_gain, diff_lam_q1, diff_lam_k1, diff_lam_q2, diff_lam_k2,
           diff_sub_gain, diff_w_o,
           moe_w_grp, moe_b_grp, moe_w_exp, moe_b_exp, moe_w_gate, moe_w_up, moe_w_down):
    f32 = np.float32
    A = lambda a: np.asarray(a, dtype=f32)
    x, c, ctx, c_ctx = A(x), A(c), A(ctx), A(c_ctx)
    depth = 4
    NC = NCORES
    c_all = np.concatenate([c, c_ctx[None]], 0)
    cT = _c(c_all.reshape(5, KC, 128).transpose(2, 1, 0))
    ada_w, ada_b = A(ada_w), A(ada_b)
    maps = []
    for ci in range(NC):
        sl = slice(ci * ADA_COLS, (ci + 1) * ADA_COLS)
        maps.append({"cT": cT, "w": _c(ada_w[:, :, sl]),
                     "b": _c(np.broadcast_to(ada_b[:, None, sl], (depth, 5, ADA_COLS)))})
    res = launch(build_ada(depth), maps)
    mod_full = np.concatenate([r["mod"] for r in res], axis=-1)
    del maps, res

    def modv_for(i, b):
        ml = mod_full[i][[b, 4]].reshape(12, D)
        return vfm(ml)

    def g2_for(i, b):
        return vfm(mod_full[i][[b, 4]].reshape(2, 6, D)[:, 5, :])

    def ng_for(i):
        return vfm(np.stack([A(norm1_g)[i], A(norm2_g)[i]], 0))

    def router_in(i):
        w_r = np.concatenate([A(moe_w_grp)[i], A(moe_w_exp)[i]], 1)
        b_r = np.concatenate([A(moe_b_grp)[i], A(moe_b_exp)[i]], 0)
        return {"w_r": _c(w_r.reshape(KC, 128, 36).transpose(1, 0, 2)),
                "b_r": _c(np.broadcast_to(b_r[None], (128, 36)))}

    def moe(i, mx_list, comb_list):
        TA = sum(m.shape[2] for m in mx_list)
        mx_all = _c(np.concatenate(mx_list, 2))
        comb_all = np.concatenate(comb_list, 0)
        wg = A(moe_w_gate)[i].reshape(32, D, FE)
        wu = A(moe_w_up)[i].reshape(32, D, FE)
        wd = A(moe_w_down)[i].reshape(32, FE, D)
        maps = [{"mx": mx_all, "combT": _c(comb_all[:, 4 * ci:4 * ci + 4].T),
                 "wg": _c(wg[4 * ci:4 * ci + 4]), "wu": _c(wu[4 * ci:4 * ci + 4]), "wd": _c(wd[4 * ci:4 * ci + 4])}
                for ci in range(NC)]
        res = launch(build_moe(TA), maps)
        return [r["part"] for r in res]

    def parts_for(parts, ci, T):
        return _c(np.stack([pp[:, :, ci * T:(ci + 1) * T] for pp in parts], 0))

    h_list = []
    for ci in range(NC):
        b, hf = ci // 2, ci % 2
        tok = np.concatenate([x[b, hf * 1024:(hf + 1) * 1024], ctx[b, hf * 128:(hf + 1) * 128]], 0)
        h_list.append(fm(tok))

    def gmlp_layer(i, j, T, h_list, parts):
        common = {"ng": ng_for(i), "w_in": A(gmlp_w_in)[j], "w_out": A(gmlp_w_out)[j], "vgain": vfm(A(gmlp_v_gain)[j]),
                  "wsT": _c(A(gmlp_w_s)[j].transpose(2, 0, 1)),
                  "bsb": _c(np.broadcast_to(A(gmlp_b_s)[j][None], (128, 16, 128)))}
        common.update(router_in(i))
        maps = []
        for ci in range(NC):
            m = dict(common)
            m["h_in"] = h_list[ci]
            m["modv"] = modv_for(i, ci // 2)
            if parts is not None:
                m["part"] = parts_for(parts, ci, T)
                m["g2prev"] = g2_for(i - 1, ci // 2)
            maps.append(m)
        res = launch(build_gmlp(T, 0 if parts is None else NC), maps)
        return [r["h1"] for r in res], [r["mx"] for r in res], [r["comb"] for r in res]

    def attn_layer(kind, i, h_list, parts, TQ):
        cfg = ATT[kind]
        if kind == "gqa":
            w_qkv, w_o = A(gqa_w_qkv)[0], A(gqa_w_o)[0]
            gains = np.stack([A(gqa_q_gain)[0], A(gqa_k_gain)[0]], 1)
        else:
            w_qkv, w_o = A(diff_w_qkv)[0], A(diff_w_o)[0]
            gains = np.stack([A(diff_q_gain)[0].reshape(128), A(diff_k_gain)[0].reshape(128)], 1)
        maps = []
        for ci in range(NC):
            cosT, sinT, rm, onesn = rope_tables(cfg["dh"], ci % 2)
            maps.append({"h_in": h_list[ci], "modv": modv_for(i, ci // 2), "ng": ng_for(i), "w_qkv": w_qkv,
                         "qk_gain": _c(gains), "onesn": onesn, "rotm": rm, "cosT": cosT, "sinT": sinT,
                         "part": parts_for(parts, ci, 1152), "g2prev": g2_for(i - 1, ci // 2)})
        resA = launch(build_attn_a(kind, NC), maps)
        del maps
        mapsB = []
        rin = router_in(i)
        for ci in range(NC):
            e, o = (ci // 2) * 2, (ci // 2) * 2 + 1
            m = {"h_in": _c(resA[ci]["h0"][:, :, :TQ]), "modv": modv_for(i, ci // 2), "ng": ng_for(i),
                 "qT": _c(resA[ci]["qT"][:, :, :TQ]),
                 "kT": _c(np.concatenate([resA[e]["kT"], resA[o]["kT"]], 2)),
                 "v": _c(np.concatenate([resA[e]["v"], resA[o]["v"]], 0)), "w_o": w_o}
            m.update(rin)
            if kind == "diff":
                lq = np.stack([A(diff_lam_q1)[0], A(diff_lam_k1)[0], A(diff_lam_q2)[0], A(diff_lam_k2)[0]], 0)
                lam_init = 0.8 - 0.6 * float(np.exp(-0.3 * i))
                m["lamv"] = _c(np.broadcast_to(lq[None], (128, 4, 64)))
                m["subg"] = _c(np.stack([A(diff_sub_gain)[0], np.full(128, lam_init, f32)], 1).astype(f32))
            mapsB.append(m)
        resB = launch(build_attn_b(kind, TQ), mapsB)
        h0_ctx = [r["h0"] for r in resA]
        return [r["h1"] for r in resB], [r["mx"] for r in resB], [r["comb"] for r in resB], h0_ctx

    h1, mx, comb = gmlp_layer(0, 0, 1152, h_list, None)
    parts = moe(0, mx, comb)
    h1, mx, comb, _ = attn_layer("gqa", 1, h1, parts, 1152)
    parts = moe(1, mx, comb)
    h1, mx, comb, _ = attn_layer("diff", 2, h1, parts, 1024)
    parts = moe(2, mx, comb)
    h1, mx, comb = gmlp_layer(3, 1, 1024, h1, parts)
    parts = moe(3, mx, comb)
    maps = [{"h_in": h1[ci], "modv": modv_for(3, ci // 2), "ng": ng_for(3), "part": parts_for(parts, ci, 1024),
             "g2prev": g2_for(3, ci // 2)} for ci in range(NC)]
    res = launch(build_final(1024, NC), maps)
    out = np.zeros((4, 2048, D), f32)
    for ci in range(NC):
        b, hf = ci // 2, ci % 2
        out[b, hf * 1024:(hf + 1) * 1024] = unfm(res[ci]["hout"])
    return out
```
